# Optimizing a Trainium2 kernel written in Bass

```python
import math
import jax, jax.numpy as jnp
from jax import lax
import numpy as np

D_MODEL = 1024
BATCH = 8
SEQ = 8192
DEPTH = 1

MEM_LEN = 256
SSD_HEAD_DIM = 64
SSD_WIDTH = D_MODEL // 2
SSD_HEADS = SSD_WIDTH // SSD_HEAD_DIM
SSD_GROUPS = 2
SSD_HEADS_PER_GROUP = SSD_HEADS // SSD_GROUPS
SSD_STATE = 64
SSD_CONV = 4
SSD_CHUNK = 128
SC_WIDTH = D_MODEL - SSD_WIDTH
SC_CONV = 3
MIX_WIDTH = SSD_WIDTH + SC_WIDTH
XBC_WIDTH = SSD_WIDTH + 2 * SSD_GROUPS * SSD_STATE
IN_COLS = SSD_WIDTH + XBC_WIDTH + SSD_HEADS + 3 * SC_WIDTH
XA_HEADS = 4
XA_HEAD_DIM = D_MODEL // XA_HEADS
N_GROUPS_MOE = 4
EXPERTS_PER_GROUP = 8
N_EXPERTS = N_GROUPS_MOE * EXPERTS_PER_GROUP
TOP_K = 2
D_EXPERT = D_MODEL // 2
MOE_BLOCK = 128
EPS = 1e-6

kernel_name = 'hybrid_ssd_shortconv_memxattn_hmoe'


def rms_norm(x, g):
    xf = x.astype(jnp.float32)
    y = xf * lax.rsqrt(jnp.mean(xf * xf, axis=-1, keepdims=True) + EPS)
    return (y * g.astype(jnp.float32)).astype(x.dtype)


def causal_dwconv(u, w):
    k = w.shape[0]
    s = u.shape[1]
    up = jnp.pad(u, ((0, 0), (k - 1, 0), (0, 0)))
    out = up[:, 0:s] * w[0]
    for i in range(1, k):
        out = out + up[:, i:i + s] * w[i]
    return out


def ssd_chunked(x, dt, a, bm, cm):
    f32 = jnp.float32
    bsz, s, _ = x.shape
    nc = s // SSD_CHUNK
    G, R, P, N, Q = SSD_GROUPS, SSD_HEADS_PER_GROUP, SSD_HEAD_DIM, SSD_STATE, SSD_CHUNK
    xc = x.astype(f32).reshape(bsz, nc, Q, G, R, P)
    dtc = dt.astype(f32).reshape(bsz, nc, Q, G, R)
    bc = bm.astype(f32).reshape(bsz, nc, Q, G, N)
    cc = cm.astype(f32).reshape(bsz, nc, Q, G, N)
    a_dt = (dtc * a.astype(f32).reshape(G, R)).transpose(0, 3, 4, 1, 2)
    cs = jnp.cumsum(a_dt, axis=-1)
    xdt = xc * dtc[..., None]
    causal = jnp.tril(jnp.ones((Q, Q), dtype=bool))
    decay = jnp.exp(jnp.where(causal, cs[..., :, None] - cs[..., None, :], -jnp.inf))
    cb = jnp.einsum('bclgn,bcsgn->bgcls', cc, bc)
    y_diag = jnp.einsum('bgcls,bgrcls,bcsgrp->bclgrp', cb, decay, xdt)
    decay_in = jnp.exp(cs[..., -1:] - cs)
    states = jnp.einsum('bclgn,bgrcl,bclgrp->bcgrpn', bc, decay_in, xdt)
    chunk_decay = jnp.exp(cs[..., -1])

    def step(carry, inp):
        st, dec = inp
        return carry * dec[..., None, None] + st, carry

    init = jnp.zeros((bsz, G, R, P, N), states.dtype)
    _, prev = lax.scan(step, init, (states.transpose(1, 0, 2, 3, 4, 5), chunk_decay.transpose(3, 0, 1, 2)))
    prev = prev.transpose(1, 0, 2, 3, 4, 5)
    y_off = jnp.einsum('bclgn,bcgrpn,bgrcl->bclgrp', cc, prev, jnp.exp(cs))
    return (y_diag + y_off).reshape(bsz, s, G * R * P).astype(x.dtype)


def hybrid_mixer(n, w_in, conv_ssd_w, conv_ssd_b, dt_bias, a_log, d_skip, norm_ssd_gate, conv_short_w, w_out):
    proj = n @ w_in
    o1 = SSD_WIDTH
    o2 = o1 + XBC_WIDTH
    o3 = o2 + SSD_HEADS
    o4 = o3 + SC_WIDTH
    o5 = o4 + SC_WIDTH
    z, xbc, dt_raw, g_b, g_c, v = jnp.split(proj, [o1, o2, o3, o4, o5], axis=-1)
    xbc = jax.nn.silu(causal_dwconv(xbc, conv_ssd_w) + conv_ssd_b)
    xs, bm, cm = jnp.split(xbc, [SSD_WIDTH, SSD_WIDTH + SSD_GROUPS * SSD_STATE], axis=-1)
    dt = jax.nn.softplus(dt_raw + dt_bias)
    a = -jnp.exp(a_log)
    y = ssd_chunked(xs, dt, a, bm, cm) + xs * jnp.repeat(d_skip, SSD_HEAD_DIM)
    gated = (y * jax.nn.silu(z)).reshape(y.shape[:-1] + (SSD_GROUPS, SSD_WIDTH // SSD_GROUPS))
    y_ssd = rms_norm(gated, norm_ssd_gate.reshape(SSD_GROUPS, SSD_WIDTH // SSD_GROUPS)).reshape(y.shape)
    y_sc = g_b * causal_dwconv(g_c * v, conv_short_w)
    return jnp.concatenate([y_ssd, y_sc], axis=-1) @ w_out


def memory_cross_attention(n, mem_n, w_q, w_kv, w_o):
    bsz, s, d = n.shape
    m = mem_n.shape[1]
    q = (n @ w_q).reshape(bsz, s, XA_HEADS, XA_HEAD_DIM)
    k, v = jnp.split(mem_n @ w_kv, 2, axis=-1)
    k = k.reshape(bsz, m, XA_HEADS, XA_HEAD_DIM)
    v = v.reshape(bsz, m, XA_HEADS, XA_HEAD_DIM)
    scores = jnp.einsum('bshd,bmhd->bhsm', q, k).astype(jnp.float32) * (XA_HEAD_DIM ** -0.5)
    p = jax.nn.softmax(scores, axis=-1).astype(v.dtype)
    o = jnp.einsum('bhsm,bmhd->bshd', p, v).reshape(bsz, s, d)
    return o @ w_o


def hierarchical_moe(n, w_rg, b_rg, w_re, b_re, w_gate, w_up, w_down):
    bsz, s, d = n.shape
    t = n.reshape(-1, d)
    T = t.shape[0]
    f32 = jnp.float32
    g_prob = jax.nn.softmax((t @ w_rg).astype(f32) + b_rg.astype(f32), axis=-1)
    g_idx = jnp.argmax(g_prob, axis=-1).astype(jnp.int32)
    g_w = jnp.take_along_axis(g_prob, g_idx[:, None], axis=-1)
    e_logits = ((t @ w_re).astype(f32) + b_re.astype(f32)).reshape(T, N_GROUPS_MOE, EXPERTS_PER_GROUP)
    e_logits = jnp.take_along_axis(e_logits, g_idx[:, None, None], axis=1)[:, 0]
    e_prob = jax.nn.softmax(e_logits, axis=-1)
    top_w, top_i = lax.top_k(e_prob, TOP_K)
    top_w = top_w / jnp.sum(top_w, axis=-1, keepdims=True)
    comb = (g_w * top_w).reshape(-1)
    flat_e = (g_idx[:, None] * EXPERTS_PER_GROUP + top_i.astype(jnp.int32)).reshape(-1)
    n_assign = T * TOP_K
    n_blocks = (n_assign + MOE_BLOCK - 1) // MOE_BLOCK + N_EXPERTS
    order = jnp.argsort(flat_e)
    tok_of = order // TOP_K
    sorted_e = flat_e[order]
    sizes = jnp.bincount(flat_e, length=N_EXPERTS)
    padded = ((sizes + MOE_BLOCK - 1) // MOE_BLOCK) * MOE_BLOCK
    pad_end = jnp.cumsum(padded)
    pad_start = pad_end - padded
    seg_start = jnp.cumsum(sizes) - sizes
    dest = pad_start[sorted_e] + (jnp.arange(n_assign) - seg_start[sorted_e])
    buf = jnp.zeros((n_blocks * MOE_BLOCK, d), t.dtype).at[dest].set(t[tok_of])
    block_expert = jnp.clip(jnp.searchsorted(pad_end, jnp.arange(n_blocks) * MOE_BLOCK, side='right'), 0, N_EXPERTS - 1)

    def expert_block(args):
        xb, e = args
        return (jax.nn.silu(xb @ w_gate[e]) * (xb @ w_up[e])) @ w_down[e]

    y_buf = lax.map(expert_block, (buf.reshape(n_blocks, MOE_BLOCK, d), block_expert))
    y_sorted = y_buf.reshape(-1, d)[dest] * comb[order][:, None].astype(t.dtype)
    out = jnp.zeros_like(t).at[tok_of].add(y_sorted)
    return out.reshape(bsz, s, d)


def setup_inputs(seed: int = 0) -> dict:
    key = jax.random.key(seed)
    ks = jax.random.split(key, 26)
    L, D = DEPTH, D_MODEL

    def nrm(k, shape, scale):
        return jax.random.normal(k, shape, jnp.float32) * scale

    def gain(k, shape):
        return 1.0 + 0.02 * jax.random.normal(k, shape, jnp.float32)

    dt0 = jnp.exp(jax.random.uniform(ks[7], (L, SSD_HEADS), jnp.float32, minval=math.log(1e-3), maxval=math.log(1e-1)))
    return {
        'x': nrm(ks[0], (BATCH, SEQ, D), 1.0),
        'mem': nrm(ks[1], (BATCH, MEM_LEN, D), 1.0),
        'norm_mem': gain(ks[2], (D,)),
        'norm_mix': gain(ks[3], (L, D)),
        'w_in': nrm(ks[4], (L, D, IN_COLS), D ** -0.5),
        'conv_ssd_w': nrm(ks[5], (L, SSD_CONV, XBC_WIDTH), SSD_CONV ** -0.5),
        'conv_ssd_b': nrm(ks[6], (L, XBC_WIDTH), 0.02),
        'dt_bias': dt0 + jnp.log(-jnp.expm1(-dt0)),
        'a_log': jnp.log(jax.random.uniform(ks[8], (L, SSD_HEADS), jnp.float32, minval=1.0, maxval=16.0)),
        'd_skip': 1.0 + 0.1 * jax.random.normal(ks[9], (L, SSD_HEADS), jnp.float32),
        'norm_ssd_gate': gain(ks[10], (L, SSD_WIDTH)),
        'conv_short_w': nrm(ks[11], (L, SC_CONV, SC_WIDTH), SC_CONV ** -0.5),
        'w_out': nrm(ks[12], (L, MIX_WIDTH, D), MIX_WIDTH ** -0.5),
        'norm_xattn': gain(ks[13], (L, D)),
        'w_q': nrm(ks[14], (L, D, D), D ** -0.5),
        'w_kv': nrm(ks[15], (L, D, 2 * D), D ** -0.5),
        'w_o': nrm(ks[16], (L, D, D), D ** -0.5),
        'norm_moe': gain(ks[17], (L, D)),
        'w_router_group': nrm(ks[18], (L, D, N_GROUPS_MOE), D ** -0.5),
        'b_router_group': nrm(ks[19], (L, N_GROUPS_MOE), 0.01),
        'w_router_expert': nrm(ks[20], (L, D, N_EXPERTS), D ** -0.5),
        'b_router_expert': nrm(ks[21], (L, N_EXPERTS), 0.01),
        'w_gate': nrm(ks[22], (L, N_EXPERTS, D, D_EXPERT), D ** -0.5),
        'w_up': nrm(ks[23], (L, N_EXPERTS, D, D_EXPERT), D ** -0.5),
        'w_down': nrm(ks[24], (L, N_EXPERTS, D_EXPERT, D), D_EXPERT ** -0.5),
        'norm_final': gain(ks[25], (D,)),
    }


def reference(x, mem, norm_mem, norm_mix, w_in, conv_ssd_w, conv_ssd_b, dt_bias, a_log, d_skip,
              norm_ssd_gate, conv_short_w, w_out, norm_xattn, w_q, w_kv, w_o, norm_moe,
              w_router_group, b_router_group, w_router_expert, b_router_expert,
              w_gate, w_up, w_down, norm_final):
    mem_n = rms_norm(mem, norm_mem)
    h = x
    for l in range(DEPTH):
        h = h + hybrid_mixer(rms_norm(h, norm_mix[l]), w_in[l], conv_ssd_w[l], conv_ssd_b[l],
                             dt_bias[l], a_log[l], d_skip[l], norm_ssd_gate[l], conv_short_w[l], w_out[l])
        h = h + memory_cross_attention(rms_norm(h, norm_xattn[l]), mem_n, w_q[l], w_kv[l], w_o[l])
        h = h + hierarchical_moe(rms_norm(h, norm_moe[l]), w_router_group[l], b_router_group[l],
                                 w_router_expert[l], b_router_expert[l], w_gate[l], w_up[l], w_down[l])
    return rms_norm(h, norm_final)
```

```python
import numpy as np
import concourse.bass as bass
import concourse.mybir as mybir
from concourse.bass_utils import run_bass_kernel_spmd

F32 = mybir.dt.float32
BF16 = mybir.dt.bfloat16
I32 = mybir.dt.int32
ALU = mybir.AluOpType
AF = mybir.ActivationFunctionType
AX = mybir.AxisListType

D = 1024
MEM = 256
NEXP = 32
DE = 512
IN_COLS = 2824
EPS = 1e-6


class T:
    __slots__ = ("ap", "name", "w", "r", "rd", "ep", "root")

    def __init__(self, ap, name="", base=None):
        self.ap = ap
        self.name = name
        self.w = None
        self.r = {}
        self.rd = []
        self.ep = -1
        self.root = base.root if base is not None else self

    def __getitem__(self, k):
        return self.ap[k]


class Op:
    __slots__ = ("eng", "fn", "dma", "deps", "sig", "sigval", "dsem", "dval", "prev", "ep")

    def __init__(self, eng, fn, dma, ep):
        self.eng = eng
        self.fn = fn
        self.dma = dma
        self.deps = []
        self.sig = False
        self.sigval = 0
        self.dsem = None
        self.dval = 0
        self.prev = None
        self.ep = ep


class _Rec:
    def __init__(self):
        self.call = None

    def __getattr__(self, name):
        def f(*a, **k):
            self.call = (name, a, k)
            return None
        return f


class Sched:
    DMA_POOL = {"sp": 24, "act": 4, "pool": 16}

    def __init__(self, nc):
        self.nc = nc
        self.ops = []
        self.engs = {"pe": nc.tensor, "act": nc.scalar, "dve": nc.vector,
                     "pool": nc.gpsimd, "sp": nc.sync}
        self._n = 0
        self.ep = 0
        self.esem = {k: nc.alloc_semaphore(f"s_{k}") for k in ("pe", "act", "dve", "pool")}
        self.pools = {q: [nc.alloc_semaphore(f"d_{q}{i}") for i in range(n)]
                      for q, n in self.DMA_POOL.items()}
        self.cnt = {k: 0 for k in self.esem}
        self.dcnt = {q: 0 for q in self.pools}
        self.hist = {q: [] for q in self.pools}
        self.waited = {k: {} for k in self.engs}
        self.scopes = []
        self.nwait = 0
        self.nops = 0
        self.maxops = None
        self.nrec = 0

    def push(self):
        self.scopes.append([])

    def pop(self):
        for g in reversed(self.scopes.pop()):
            g.__exit__(None, None, None)

    def sb(self, shape, dtype, name="sb"):
        self._n += 1
        g = self.nc.sbuf_tensor(f"{name}_{self._n}", list(shape), dtype)
        h = g.__enter__()
        self.scopes[-1].append(g)
        return T(h.ap(), name)

    def ps(self, shape, dtype, name="ps"):
        self._n += 1
        g = self.nc.psum_tensor(f"{name}_{self._n}", list(shape), dtype)
        h = g.__enter__()
        self.scopes[-1].append(g)
        return T(h.ap(), name)

    def dram(self, shape, dtype, name):
        return T(self.nc.dram_tensor(name, list(shape), dtype).ap(), name)

    def _chk(self, t):
        if t.ep != self.ep:
            t.w = None
            t.r = {}
            t.rd = []
            t.ep = self.ep

    def op(self, eng, fn, reads=(), writes=(), dma=False):
        self.nrec += 1
        if self.maxops is not None and self.nrec > self.maxops:
            return None
        rec = _Rec()
        fn(rec)
        assert rec.call is not None
        o = Op(eng, rec.call, dma, self.ep)
        reads = [t.root for t in reads]
        writes = [t.root for t in writes]
        raw = []
        war = []
        for t in reads:
            self._chk(t)
            if t.w is not None:
                raw.append(t.w)
        for t in writes:
            self._chk(t)
            if t.w is not None:
                raw.append(t.w)
            war.extend(t.r.values())
            war.extend(t.rd)
        deps = []
        seen = set()
        for d in raw:
            if id(d) in seen:
                continue
            seen.add(id(d))
            if (not d.dma) and (not dma) and d.eng == eng and eng == "pe":
                continue
            deps.append(d)
        for d in war:
            if id(d) in seen:
                continue
            seen.add(id(d))
            if (not d.dma) and (not dma) and d.eng == eng and eng == "pe":
                continue
            deps.append(d)
        o.deps = deps
        for t in reads:
            if dma:
                t.rd.append(o)
            else:
                t.r[eng] = o
        for t in writes:
            t.w = o
            t.r = {}
            t.rd = []
        self.ops.append(o)
        return o

    def dma(self, q, out_t, out_ap, in_t, in_ap, extra_reads=(), **kw):
        reads = [t for t in (in_t,) if t is not None] + list(extra_reads)
        writes = [t for t in (out_t,) if t is not None]
        return self.op(q, lambda e: e.dma_start(out=out_ap, in_=in_ap, **kw),
                       reads=reads, writes=writes, dma=True)

    def flush(self):
        ops = self.ops
        self.ops = []
        last = {}
        for o in ops:
            for d in o.deps:
                d.sig = True
            if not o.dma:
                last[o.eng] = o
        for o in last.values():
            o.sig = True
        for o in ops:
            if o.dma:
                K = len(self.pools[o.eng])
                i = self.dcnt[o.eng]
                o.dsem = self.pools[o.eng][i % K]
                o.dval = 16 * (i // K + 1)
                o.prev = self.hist[o.eng][i - K] if i >= K else None
                self.hist[o.eng].append(o)
                self.dcnt[o.eng] = i + 1
            elif o.sig:
                self.cnt[o.eng] += 1
                o.sigval = self.cnt[o.eng]
        for o in ops:
            e = self.engs[o.eng]
            needs = []
            for d in o.deps:
                if d.dma:
                    needs.append((d.dsem, d.dval))
                else:
                    needs.append((self.esem[d.eng], d.sigval))
            if o.dma and o.prev is not None:
                needs.append((o.prev.dsem, o.prev.dval))
            w = self.waited[o.eng]
            for sem, val in needs:
                key = id(sem)
                if w.get(key, 0) < val:
                    e.wait_ge(sem, val)
                    w[key] = val
                    self.nwait += 1
            name, a, k = o.fn
            inst = getattr(e, name)(*a, **k)
            if o.dma:
                inst.then_inc(o.dsem, 16)
            elif o.sig:
                inst.then_inc(self.esem[o.eng], 1)
        self.nops += len(ops)
        targets = [(self.esem[k], self.cnt[k]) for k in self.esem if self.cnt[k] > 0]
        for q, lst in self.hist.items():
            lastv = {}
            for o in lst[-len(self.pools[q]):]:
                lastv[id(o.dsem)] = (o.dsem, o.dval)
            targets.extend(lastv.values())
        for k, e in self.engs.items():
            w = self.waited[k]
            for sem, val in targets:
                if k in self.esem and sem is self.esem[k]:
                    continue
                if w.get(id(sem), 0) < val:
                    e.wait_ge(sem, val)
                    w[id(sem)] = val
        self.ep += 1


def _pack(items):
    offs = {}
    cols = []
    o = 0
    for name, arr in items:
        arr = np.ascontiguousarray(arr, dtype=np.float32).reshape(128, -1)
        offs[name] = (o, arr.shape[1])
        cols.append(arr)
        o += arr.shape[1]
    return offs, np.ascontiguousarray(np.concatenate(cols, axis=1))


def _bc(v):
    v = np.asarray(v, dtype=np.float32).reshape(-1)
    return np.broadcast_to(v[None, :], (128, v.shape[0]))


def _colT(v, nchunk):
    return np.asarray(v, dtype=np.float32).reshape(nchunk, 128).T


def pack_params(inp, CAP):
    l = 0
    items = [
        ("g1T", _colT(inp["norm_mix"][l], 8)),
        ("g2T", _colT(inp["norm_xattn"][l], 8)),
        ("gmT", _colT(inp["norm_mem"], 8)),
        ("ggT", _colT(inp["norm_ssd_gate"][l], 4)),
        ("cw", np.asarray(inp["conv_ssd_w"][l]).T.reshape(6, 128, 4).transpose(1, 0, 2)),
        ("cb", _colT(inp["conv_ssd_b"][l], 6)),
        ("sw", np.asarray(inp["conv_short_w"][l]).T.reshape(4, 128, 3).transpose(1, 0, 2)),
        ("dsk", _colT(np.repeat(np.asarray(inp["d_skip"][l]), 64), 4)),
        ("dtb", _bc(inp["dt_bias"][l])),
        ("alog", _bc(inp["a_log"][l])),
        ("br", _bc(np.concatenate([np.asarray(inp["b_router_group"][l]), np.asarray(inp["b_router_expert"][l])]))),
        ("ident", np.eye(128, dtype=np.float32)),
        ("tri", np.triu(np.ones((128, 128), np.float32))),
        ("tris", np.triu(np.ones((128, 128), np.float32), 1)),
        ("ones", np.ones((128, 128), np.float32)),
        ("eidx", _bc(np.arange(32))),
        ("pidx", np.arange(128, dtype=np.float32).reshape(128, 1)),
    ]
    return _pack(items)


def build(SEQ, TS, CB, offs, NPRM, dbg=False, stop=None, BURST=2):
    NS = TS // 128
    NST = SEQ // TS
    NT = SEQ // 128
    CAP = CB * 128
    NSLOT = NEXP * CAP
    NB = NEXP * CB

    nc = bass.Bass("TRN2", target_bir_lowering=False)
    S = Sched(nc)
    import os as _os
    if _os.environ.get("K_MAXOPS"):
        S.maxops = int(_os.environ["K_MAXOPS"])

    def din(name, shape, dt=F32):
        return nc.dram_tensor(name, list(shape), dt, kind="ExternalInput").ap()

    x_d = din("x", [SEQ, D])
    mem_d = din("mem", [MEM, D])
    prm_d = din("prm", [128, NPRM])
    w_in_d = din("w_in", [D, IN_COLS])
    w_out_d = din("w_out", [D, D])
    w_q_d = din("w_q", [D, D])
    w_kv_d = din("w_kv", [D, 2 * D])
    w_o_d = din("w_o", [D, D])
    w_r_d = din("w_r", [D, 36])
    w_g_d = din("w_gate", [NEXP, D, DE])
    w_u_d = din("w_up", [NEXP, D, DE])
    w_d_d = din("w_down", [NEXP, DE, D])
    g3_d = din("g3b", [128, D])
    gF_d = din("gFb", [128, D])
    out_d = nc.dram_tensor("out", [SEQ, D], F32, kind="ExternalOutput").ap()
    if dbg:
        dbg_h1 = nc.dram_tensor("dbg_h1", [SEQ, D], F32, kind="ExternalOutput").ap()
        dbg_h2 = nc.dram_tensor("dbg_h2", [SEQ, D], F32, kind="ExternalOutput").ap()

    h2buf = S.dram([SEQ, D], F32, "h2buf")
    n3buf = S.dram([SEQ, D], BF16, "n3buf")
    ybuf = S.dram([NSLOT + 128, D], F32, "ybuf")
    tab = S.dram([128 * NB + 128, 2], I32, "tab")

    S.push()
    PRM = S.sb([128, NPRM], F32, "prm")

    def P(name, a=None, b=None):
        o, n = offs[name]
        if a is None:
            return PRM[:, o:o + n]
        return PRM[:, o + a:o + b]

    ROWS = S.sb([128, NT, 2], I32, "rows")
    identb = S.sb([128, 128], BF16, "identb")
    onesb = S.sb([128, 128], BF16, "onesb")
    trisb = S.sb([128, 128], BF16, "trisb")
    trib = S.sb([128, 128], BF16, "trib")

    S.dma("sp", PRM, PRM[:], None, prm_d[:, :])
    S.op("dve", lambda e: e.tensor_copy(out=identb[:], in_=P("ident")), reads=[PRM], writes=[identb])
    S.op("dve", lambda e: e.tensor_copy(out=onesb[:], in_=P("ones")), reads=[PRM], writes=[onesb])
    S.op("dve", lambda e: e.tensor_copy(out=trisb[:], in_=P("tris")), reads=[PRM], writes=[trisb])
    S.op("dve", lambda e: e.tensor_copy(out=trib[:], in_=P("tri")), reads=[PRM], writes=[trib])

    def rstd_from_ss(ss, out, n, width=1):
        S.op("dve", lambda e: e.tensor_scalar(out=out[:, 0:width], in0=ss[:, 0:width], scalar1=1.0 / n, scalar2=EPS,
                                              op0=ALU.mult, op1=ALU.add), reads=[ss], writes=[out])
        S.op("act", lambda e: e.activation(out=out[:, 0:width], in_=out[:, 0:width], func=AF.Ln), reads=[out], writes=[out])
        S.op("act", lambda e: e.activation(out=out[:, 0:width], in_=out[:, 0:width], func=AF.Exp, scale=-0.5), reads=[out], writes=[out])

    S.push()
    W_in = S.sb([128, 8, IN_COLS], BF16, "w_in")
    W_out = S.sb([128, 8, D], BF16, "w_out")
    W_q = S.sb([128, 8, D], BF16, "w_q")
    W_o = S.sb([128, 8, D], BF16, "w_o")
    W_r = S.sb([128, 8, 36], F32, "w_r")
    KT = S.sb([128, 8, MEM], BF16, "KT")
    V = S.sb([128, 2, D], BF16, "V")
    G3 = S.sb([128, D], F32, "g3")
    DRV = S.sb([128, 64], F32, "drv")
    S.dma("pool", W_in, W_in[:], None, w_in_d.rearrange("(c p) n -> p c n", p=128))
    S.dma("sp", W_r, W_r[:], None, w_r_d.rearrange("(c p) n -> p c n", p=128))
    W_rh = S.sb([128, 8, 36], BF16, "w_rh")
    W_rl = S.sb([128, 8, 36], BF16, "w_rl")
    S.op("dve", lambda e: e.tensor_copy(out=W_rh[:], in_=W_r[:]), reads=[W_r], writes=[W_rh])
    S.op("dve", lambda e: e.tensor_tensor(out=W_rl[:], in0=W_r[:], in1=W_rh[:], op=ALU.subtract), reads=[W_r, W_rh], writes=[W_rl])
    S.dma("sp", G3, G3[:], None, g3_d[:, :])
    S.op("dve", lambda e: e.tensor_scalar(out=DRV[:, 0:24], in0=P("cw"), scalar1=0.5, scalar2=None, op0=ALU.mult),
         reads=[PRM], writes=[DRV])
    S.op("dve", lambda e: e.tensor_scalar(out=DRV[:, 24:30], in0=P("cb"), scalar1=0.5, scalar2=None, op0=ALU.mult),
         reads=[PRM], writes=[DRV])
    S.op("act", lambda e: e.activation(out=DRV[:, 32:40], in_=P("alog"), func=AF.Exp), reads=[PRM], writes=[DRV])
    S.op("dve", lambda e: e.tensor_scalar(out=DRV[:, 48:49], in0=P("pidx"), scalar1=float(128 * NB), scalar2=None, op0=ALU.add),
         reads=[PRM], writes=[DRV])
    S.op("dve", lambda e: e.tensor_scalar(out=DRV[:, 32:40], in0=DRV[:, 32:40], scalar1=-1.0, scalar2=None, op0=ALU.mult),
         reads=[DRV], writes=[DRV])

    S.push()
    W_kv = S.sb([128, 8, 2 * D], BF16, "w_kv")
    mt = S.sb([128, 2, D], F32, "mt")
    mnT = S.sb([128, 8, MEM], BF16, "mnT")
    zrow = S.sb([128, D], F32, "zrow")
    xn0 = S.sb([128, D], BF16, "xn0")
    ss0 = S.sb([128, 1], F32, "ss0")
    rs0 = S.sb([128, 1], F32, "rs0")
    P0 = [S.ps([128, 512], F32, f"p0{i}") for i in range(2)]
    PT0 = S.ps([128, 1024], BF16, "pt0")
    S.op("pool", lambda e: e.memset(zrow[:], 0.0), writes=[zrow])
    S.dma("sp", tab, tab.ap[0:128 * NB, :].rearrange("(p a) b -> p (a b)", p=128), zrow, zrow[:, 0:2 * NB].bitcast(I32))
    S.dma("sp", ybuf, ybuf.ap[NSLOT:NSLOT + 128, :], zrow, zrow[:])
    S.dma("pool", W_kv, W_kv[:], None, w_kv_d.rearrange("(c p) n -> p c n", p=128))
    S.dma("sp", mt, mt[:], None, mem_d.rearrange("(s p) d -> p s d", p=128))
    for s in range(2):
        S.op("act", lambda e, s=s: e.activation(out=xn0[:], in_=mt[:, s, :], func=AF.Square, accum_out=ss0[:, 0:1]),
             reads=[mt], writes=[xn0, ss0])
        rstd_from_ss(ss0, rs0, D)
        S.op("act", lambda e, s=s: e.activation(out=xn0[:], in_=mt[:, s, :], func=AF.Copy, scale=rs0[:, 0:1]),
             reads=[mt, rs0], writes=[xn0])
        for k in range(8):
            S.op("pe", lambda e, k=k: e.transpose(out=PT0[:, k * 128:(k + 1) * 128], in_=xn0[:, k * 128:(k + 1) * 128],
                                                  identity=identb[:]), reads=[xn0, identb], writes=[PT0])
        S.op("dve", lambda e, s=s: e.tensor_tensor(out=mnT[:, :, s * 128:(s + 1) * 128],
                                                   in0=PT0[:].rearrange("p (k t) -> p k t", k=8),
                                                   in1=P("gmT").unsqueeze(2).to_broadcast([128, 8, 128]), op=ALU.mult),
             reads=[PT0, PRM], writes=[mnT])
    pi = 0
    for c in range(8):
        ps = P0[pi % 2]
        pi += 1
        for k in range(8):
            S.op("pe", lambda e, c=c, k=k, ps=ps: e.matmul(ps[:, 0:MEM], lhsT=W_kv[:, k, c * 128:(c + 1) * 128], rhs=mnT[:, k, :],
                                                           start=(k == 0), stop=(k == 7)), reads=[W_kv, mnT], writes=[ps])
        S.op("act", lambda e, c=c, ps=ps: e.copy(out=KT[:, c, :], in_=ps[:, 0:MEM]), reads=[ps], writes=[KT])
    for mc in range(2):
        for hf in range(2):
            ps = P0[pi % 2]
            pi += 1
            for k in range(8):
                S.op("pe", lambda e, mc=mc, hf=hf, k=k, ps=ps: e.matmul(
                    ps[:, :], lhsT=mnT[:, k, mc * 128:(mc + 1) * 128], rhs=W_kv[:, k, D + hf * 512:D + (hf + 1) * 512],
                    start=(k == 0), stop=(k == 7)), reads=[W_kv, mnT], writes=[ps])
            S.op("dve", lambda e, mc=mc, hf=hf, ps=ps: e.tensor_copy(out=V[:, mc, hf * 512:(hf + 1) * 512], in_=ps[:, :]),
                 reads=[ps], writes=[V])
    S.flush()
    S.pop()
    S.dma("pool", W_out, W_out[:], None, w_out_d.rearrange("(c p) n -> p c n", p=128))
    S.dma("pool", W_q, W_q[:], None, w_q_d.rearrange("(c p) n -> p c n", p=128))
    S.dma("pool", W_o, W_o[:], None, w_o_d.rearrange("(c p) n -> p c n", p=128))

    PSA = S.ps([128, 512], F32, "psa")
    PSB = S.ps([128, 512], F32, "psb")
    PST = S.ps([128, 1024], BF16, "pst")
    PSW = S.ps([128, 1024], F32, "psw")
    PSS = S.ps([128, 512], F32, "pss")
    PSY = S.ps([128, 4, 128], F32, "psy")
    PSN = S.ps([128, 512], F32, "psn")
    PSR = PSS
    RO = 384
    rot = [PSA, PSB]
    rot_i = [0]

    def nextps():
        rot_i[0] += 1
        return rot[rot_i[0] % 2]

    XT = [S.sb([128, NS, D], F32, f"xt{i}") for i in range(2)]
    xn = S.sb([128, D], BF16, "xn")
    junk = xn
    ss = S.sb([128, 8], F32, "ss")
    rs = S.sb([128, 8], F32, "rs")
    GA = S.sb([128, 8, TS], BF16, "ga")
    GB = S.sb([128, 8, TS], BF16, "gb")
    GC = S.sb([128, 8, TS], BF16, "gc")
    xbc = S.sb([128, 6, TS + 3], F32, "xbc")
    gcv = S.sb([128, 4, TS + 2], F32, "gcv")
    gbf = S.sb([128, 4, TS], F32, "gbf")
    zs = S.sb([128, 4, TS], BF16, "zs")
    F4a = S.sb([128, 4 * TS], F32, "f4a")
    xsT = T(F4a.ap.rearrange("p (j t) -> p j t", j=4), "xsT", base=F4a)
    yT = xsT
    xsTb = S.sb([128, 4, TS], BF16, "xsTb")
    BCT = S.sb([128, 2, TS], BF16, "bct")
    CTz = S.sb([128, 2, TS], BF16, "ctz")
    tmpA = S.sb([128, TS], F32, "tmpA")
    tmpB = S.sb([128, TS], F32, "tmpB")
    tmpC = S.sb([128, TS], F32, "tmpC")
    dts = S.sb([128, NS, 8], F32, "dts")
    adt = S.sb([128, NS, 8], F32, "adt")
    adh = S.sb([128, NS, 8], BF16, "adh")
    adl = S.sb([128, NS, 8], BF16, "adl")
    sp1 = S.sb([128, NS * 8], F32, "sp1")
    sp2 = S.sb([128, NS * 8], F32, "sp2")
    css = S.sb([128, 48], F32, "css")
    F4b = S.sb([128, D], F32, "f4b")
    dif = T(F4b.ap.rearrange("p (h l) -> p h l", h=8), "dif", base=F4b)
    Dm = S.sb([128, 8, 128], BF16, "Dm")
    Mm = S.sb([128, 8, 128], BF16, "Mm")
    cbm = S.sb([128, 2, 128], F32, "cbm")
    Ecs = S.sb([128, 4, 128], BF16, "Ecs")
    CTs = S.sb([128, 4, 128], BF16, "CTs")
    xdt = S.sb([128, 512], BF16, "xdt")
    xdts = S.sb([128, 512], BF16, "xdts")
    Btok = S.sb([128, 128], BF16, "Btok")
    Sst = S.sb([128, 512], F32, "Sst")
    Sbf = [S.sb([128, 512], BF16, f"Sbf{i}") for i in range(2)]
    GA2 = S.sb([128, 8, TS], BF16, "ga2")
    GB2f = S.sb([128, 4 * TS], F32, "gb2f")
    GB2 = T(GB2f.ap.bitcast(BF16).rearrange("p (c t) -> p c t", c=8), "gb2", base=GB2f)
    xn2 = S.sb([128, D], BF16, "xn2")
    ss2 = S.sb([128, 8], F32, "ss2")
    rs2 = S.sb([128, 8], F32, "rs2")
    mx = S.sb([128, 8], F32, "mx")
    Pf = S.sb([128, 4, MEM], BF16, "Pf")
    Pn = S.sb([128, 4, MEM], BF16, "Pn")
    n3 = T(GB2f.ap[:, 0:D], "n3", base=GB2f)
    n3b = S.sb([128, D], BF16, "n3b")
    L = S.sb([128, 36], F32, "L")
    rt = S.sb([128, 160], F32, "rt")
    A0 = S.sb([128, 32], F32, "A0")
    A1 = S.sb([128, 32], F32, "A1")
    Ab = S.sb([128, 32], BF16, "Ab")
    cnt = S.sb([128, 32], F32, "cnt")
    posf = S.sb([128, 32], F32, "posf")
    pki = S.sb([128, 8], I32, "pki")
    ent = [S.sb([128, 2], I32, f"ent{i}") for i in range(4)]
    sidx = [S.sb([128, 1], I32, f"sidx{i}") for i in range(4)]

    if _os.environ.get("K_TRACE"):
        print("SBUF remaining at pass-A peak:", nc.sbuf_bytes_remaining)
    S.op("pool", lambda e: e.memset(xbc[:, :, 0:3], 0.0), writes=[xbc])
    S.op("pool", lambda e: e.memset(gcv[:, :, 0:2], 0.0), writes=[gcv])
    S.op("pool", lambda e: e.memset(Sst[:], 0.0), writes=[Sst])
    S.op("pool", lambda e: e.memset(Sbf[0][:], 0.0), writes=[Sbf[0]])
    S.op("pool", lambda e: e.memset(Sbf[1][:], 0.0), writes=[Sbf[1]])
    S.op("pool", lambda e: e.memset(CTz[:], 0.0), writes=[CTz])
    S.op("pool", lambda e: e.memset(cnt[:], 0.0), writes=[cnt])
    sbf_i = [0]

    def load_x(st):
        xt = XT[st % 2]
        S.dma("sp", xt, xt[:], None, x_d[st * TS:(st + 1) * TS, :].rearrange("(s p) d -> p s d", p=128))

    def norm_T(src_t, gname, dst, xn=xn, ss=ss, rs=rs):
        for s in range(NS):
            S.op("act", lambda e, s=s: e.activation(out=xn[:], in_=src_t[:, s, :], func=AF.Square, accum_out=ss[:, s:s + 1]),
                 reads=[src_t], writes=[xn, ss])
        rstd_from_ss(ss, rs, D, NS)
        for s in range(NS):
            S.op("act", lambda e, s=s: e.activation(out=xn[:], in_=src_t[:, s, :], func=AF.Copy, scale=rs[:, s:s + 1]),
                 reads=[src_t, rs], writes=[xn])
            for k in range(8):
                S.op("pe", lambda e, k=k: e.transpose(out=PST[:, k * 128:(k + 1) * 128], in_=xn[:, k * 128:(k + 1) * 128],
                                                      identity=identb[:]), reads=[xn, identb], writes=[PST])
            S.op("dve", lambda e, s=s: e.tensor_tensor(out=dst[:, :, s * 128:(s + 1) * 128],
                                                       in0=PST[:].rearrange("p (k t) -> p k t", k=8),
                                                       in1=P(gname).unsqueeze(2).to_broadcast([128, 8, 128]), op=ALU.mult),
                 reads=[PST, PRM], writes=[dst])

    def proj_B(Wt, c0, rhsT, ps, n=TS):
        for k in range(8):
            S.op("pe", lambda e, k=k: e.matmul(ps[:, 0:n], lhsT=Wt[:, k, c0:c0 + 128], rhs=rhsT[:, k, 0:n],
                                               start=(k == 0), stop=(k == 7)), reads=[Wt, rhsT], writes=[ps])

    def proj_A(Wt, lhsT_t, s, nk=8):
        for hf in range(2):
            for k in range(nk):
                S.op("pe", lambda e, k=k, hf=hf: e.matmul(PSW[:, hf * 512:(hf + 1) * 512], lhsT=lhsT_t[:, k, s * 128:(s + 1) * 128],
                                                          rhs=Wt[:, k, hf * 512:(hf + 1) * 512], start=(k == 0), stop=(k == nk - 1)),
                     reads=[Wt, lhsT_t], writes=[PSW])

    def gen_M(st):
        xt = XT[st % 2]
        if _os.environ.get("K_TRACE"): print("MARK", st, "norm1", S.nrec)
        norm_T(xt, "g1T", GA)
        if _os.environ.get("K_TRACE"): print("MARK", st, "inproj", S.nrec)
        for s in range(NS):
            yield
            for k in range(8):
                S.op("pe", lambda e, k=k, s=s: e.matmul(PSS[:, s * 8:(s + 1) * 8], lhsT=GA[:, k, s * 128:(s + 1) * 128],
                                                        rhs=W_in[:, k, 1280:1288], start=(k == 0), stop=(k == 7)),
                     reads=[GA, W_in], writes=[PSS])
        S.op("dve", lambda e: e.tensor_tensor(out=sp1[:].rearrange("p (s h) -> p s h", h=8),
                                              in0=PSS[:, 0:NS * 8].rearrange("p (s h) -> p s h", h=8),
                                              in1=P("dtb").unsqueeze(1).to_broadcast([128, NS, 8]), op=ALU.add),
             reads=[PSS, PRM], writes=[sp1])
        S.op("act", lambda e: e.activation(out=sp2[:], in_=sp1[:], func=AF.Abs), reads=[sp1], writes=[sp2])
        S.op("act", lambda e: e.activation(out=sp2[:], in_=sp2[:], func=AF.Exp, scale=-1.0), reads=[sp2], writes=[sp2])
        S.op("act", lambda e: e.activation(out=sp2[:], in_=sp2[:], func=AF.Ln, bias=1.0, scale=1.0), reads=[sp2], writes=[sp2])
        S.op("dve", lambda e: e.tensor_scalar(out=sp1[:], in0=sp1[:], scalar1=0.0, scalar2=None, op0=ALU.max),
             reads=[sp1], writes=[sp1])
        S.op("dve", lambda e: e.tensor_tensor(out=dts[:].rearrange("p s h -> p (s h)"), in0=sp1[:], in1=sp2[:], op=ALU.add),
             reads=[sp1, sp2], writes=[dts])
        S.op("dve", lambda e: e.tensor_tensor(out=adt[:], in0=dts[:], in1=DRV[:, 32:40].unsqueeze(1).to_broadcast([128, NS, 8]),
                                              op=ALU.mult), reads=[dts, DRV], writes=[adt])
        S.op("dve", lambda e: e.tensor_copy(out=adh[:], in_=adt[:]), reads=[adt], writes=[adh])
        S.op("dve", lambda e: e.tensor_tensor(out=adl[:], in0=adt[:], in1=adh[:], op=ALU.subtract), reads=[adt, adh], writes=[adl])
        if _os.environ.get("K_TRACE"): print("MARK", st, "z", S.nrec)
        for j in range(4):
            yield
            ps = nextps()
            proj_B(W_in, j * 128, GA, ps)
            S.op("act", lambda e, ps=ps: e.activation(out=tmpA[:], in_=ps[:, 0:TS], func=AF.Tanh, scale=0.5), reads=[ps], writes=[tmpA])
            S.op("act", lambda e, ps=ps: e.activation(out=tmpB[:], in_=ps[:, 0:TS], func=AF.Copy, scale=0.5), reads=[ps], writes=[tmpB])
            S.op("dve", lambda e, j=j: e.scalar_tensor_tensor(out=zs[:, j, :], in0=tmpA[:], scalar=1.0, in1=tmpB[:],
                                                              op0=ALU.add, op1=ALU.mult), reads=[tmpA, tmpB], writes=[zs])
        for j in range(6):
            yield
            ps = nextps()
            proj_B(W_in, 512 + j * 128, GA, ps)
            S.op("act", lambda e, ps=ps, j=j: e.copy(out=xbc[:, j, 3:3 + TS], in_=ps[:, 0:TS]), reads=[ps], writes=[xbc])
        for j in range(4):
            yield
            ps = nextps()
            proj_B(W_in, 1288 + j * 128, GA, ps)
            S.op("act", lambda e, ps=ps, j=j: e.copy(out=gbf[:, j, :], in_=ps[:, 0:TS]), reads=[ps], writes=[gbf])
        for j in range(4):
            yield
            ps = nextps()
            proj_B(W_in, 1800 + j * 128, GA, ps)
            S.op("act", lambda e, ps=ps: e.copy(out=tmpA[:], in_=ps[:, 0:TS]), reads=[ps], writes=[tmpA])
            ps2 = nextps()
            proj_B(W_in, 2312 + j * 128, GA, ps2)
            S.op("dve", lambda e, ps2=ps2, j=j: e.tensor_tensor(out=gcv[:, j, 2:2 + TS], in0=tmpA[:], in1=ps2[:, 0:TS], op=ALU.mult),
                 reads=[tmpA, ps2], writes=[gcv])
        if _os.environ.get("K_TRACE"): print("MARK", st, "conv4", S.nrec)
        for j in range(6):
            yield
            S.op("dve", lambda e, j=j: e.tensor_scalar(out=tmpA[:], in0=xbc[:, j, 0:TS], scalar1=DRV[:, j * 4:j * 4 + 1],
                                                       scalar2=DRV[:, 24 + j:25 + j], op0=ALU.mult, op1=ALU.add),
                 reads=[xbc, DRV], writes=[tmpA])
            for i in range(1, 4):
                S.op("dve", lambda e, j=j, i=i: e.scalar_tensor_tensor(out=tmpA[:], in0=xbc[:, j, i:i + TS],
                                                                       scalar=DRV[:, j * 4 + i:j * 4 + i + 1], in1=tmpA[:],
                                                                       op0=ALU.mult, op1=ALU.add), reads=[xbc, DRV, tmpA], writes=[tmpA])
            S.op("act", lambda e: e.activation(out=tmpB[:], in_=tmpA[:], func=AF.Tanh), reads=[tmpA], writes=[tmpB])
            if j < 4:
                S.op("dve", lambda e, j=j: e.scalar_tensor_tensor(out=xsT[:, j, :], in0=tmpB[:], scalar=1.0, in1=tmpA[:],
                                                                  op0=ALU.add, op1=ALU.mult), reads=[tmpA, tmpB], writes=[xsT])
                S.op("act", lambda e, j=j: e.copy(out=xsTb[:, j, :], in_=xsT[:, j, :]), reads=[xsT], writes=[xsTb])
            else:
                S.op("dve", lambda e, j=j: e.scalar_tensor_tensor(out=BCT[:, j - 4, :], in0=tmpB[:], scalar=1.0, in1=tmpA[:],
                                                                  op0=ALU.add, op1=ALU.mult), reads=[tmpA, tmpB], writes=[BCT])
        for g in range(2):
            yield
            S.op("act", lambda e, g=g: e.copy(out=CTz[g * 64:(g + 1) * 64, g, :], in_=BCT[g * 64:(g + 1) * 64, 1, :]), reads=[BCT], writes=[CTz])
        S.op("dve", lambda e: e.tensor_copy(out=xbc[:, :, 0:3], in_=xbc[:, :, TS:TS + 3]), reads=[xbc], writes=[xbc])
        if _os.environ.get("K_TRACE"): print("MARK", st, "sconv", S.nrec)
        for j in range(4):
            yield
            o = offs["sw"][0] + j * 3
            S.op("dve", lambda e, j=j, o=o: e.tensor_scalar(out=tmpC[:], in0=gcv[:, j, 0:TS], scalar1=PRM[:, o:o + 1], scalar2=None,
                                                            op0=ALU.mult), reads=[gcv, PRM], writes=[tmpC])
            for i in range(1, 3):
                S.op("dve", lambda e, j=j, i=i, o=o: e.scalar_tensor_tensor(out=tmpC[:], in0=gcv[:, j, i:i + TS],
                                                                            scalar=PRM[:, o + i:o + i + 1], in1=tmpC[:],
                                                                            op0=ALU.mult, op1=ALU.add), reads=[gcv, PRM, tmpC], writes=[tmpC])
            S.op("dve", lambda e, j=j: e.tensor_tensor(out=GB[:, 4 + j, :], in0=tmpC[:], in1=gbf[:, j, :], op=ALU.mult),
                 reads=[tmpC, gbf], writes=[GB])
        S.op("dve", lambda e: e.tensor_copy(out=gcv[:, :, 0:2], in_=gcv[:, :, TS:TS + 2]), reads=[gcv], writes=[gcv])
        if _os.environ.get("K_TRACE"): print("MARK", st, "ssd", S.nrec)
        for s in range(NS):
            yield
            sl = slice(s * 128, (s + 1) * 128)
            sprev = Sbf[sbf_i[0] % 2]
            snext = Sbf[(sbf_i[0] + 1) % 2]
            sbf_i[0] += 1
            for ii, ad in enumerate((adh, adl)):
                S.op("pe", lambda e, s=s, ad=ad, ii=ii: e.matmul(PSS[:, 32:40], lhsT=trib[:], rhs=ad[:, s, :], start=(ii == 0), stop=(ii == 1)),
                     reads=[trib, ad], writes=[PSS])
            for ii, ad in enumerate((adh, adl)):
                S.op("pe", lambda e, s=s, ad=ad, ii=ii: e.matmul(PSS[:, 40:48], lhsT=onesb[:], rhs=ad[:, s, :], start=(ii == 0), stop=(ii == 1)),
                     reads=[onesb, ad], writes=[PSS])
            for h in range(8):
                for ii, ad in enumerate((adh, adl)):
                    S.op("pe", lambda e, s=s, h=h, ad=ad, ii=ii: e.matmul(PSW[:, h * 128:(h + 1) * 128], lhsT=ad[:, s, h:h + 1].to_broadcast([128, 128]),
                                                                          rhs=trib[:], start=(ii == 0), stop=(ii == 1)), reads=[trib, ad], writes=[PSW])
            yield
            for j in range(4):
                S.op("pe", lambda e, j=j, sl=sl: e.transpose(out=PST[:, j * 128:(j + 1) * 128], in_=xsTb[:, j, sl], identity=identb[:]),
                     reads=[xsTb, identb], writes=[PST])
            S.op("pe", lambda e, sl=sl: e.transpose(out=PST[:, 512:640], in_=BCT[:, 0, sl], identity=identb[:]),
                 reads=[BCT, identb], writes=[PST])
            for g in range(2):
                S.op("pe", lambda e, g=g, sl=sl: e.matmul(PSS[:, 128 + g * 128:256 + g * 128], lhsT=BCT[:, 0, sl],
                                                          rhs=CTz[:, g, sl], start=True, stop=True),
                     reads=[BCT, CTz], writes=[PSS])
            S.op("dve", lambda e: e.tensor_copy(out=css[:, 0:8], in_=PSS[:, 32:40]), reads=[PSS], writes=[css])
            S.op("dve", lambda e: e.tensor_tensor(out=css[:, 8:16], in0=PSS[:, 40:48], in1=css[:, 0:8], op=ALU.subtract),
                 reads=[PSS, css], writes=[css])
            S.op("act", lambda e: e.activation(out=css[:, 8:16], in_=css[:, 8:16], func=AF.Exp), reads=[css], writes=[css])
            S.op("act", lambda e: e.activation(out=css[:, 24:32], in_=PSS[:, 40:48], func=AF.Exp), reads=[PSS], writes=[css])
            S.op("dve", lambda e, s=s: e.tensor_tensor(out=css[:, 16:24], in0=css[:, 8:16], in1=dts[:, s, :], op=ALU.mult),
                 reads=[css, dts], writes=[css])
            yield
            for h in range(8):
                S.op("dve", lambda e, h=h: e.tensor_scalar(out=dif[:, h, :], in0=PSW[:, h * 128:(h + 1) * 128], scalar1=css[:, h:h + 1],
                                                           scalar2=0.0, op0=ALU.subtract, op1=ALU.min), reads=[PSW, css], writes=[dif])
            S.op("act", lambda e: e.activation(out=Dm[:], in_=dif[:], func=AF.Exp), reads=[dif], writes=[Dm])
            S.op("dve", lambda e: e.tensor_tensor(out=cbm[:], in0=PSS[:, 128:384].rearrange("p (g l) -> p g l", g=2),
                                                  in1=P("tri").unsqueeze(1).to_broadcast([128, 2, 128]), op=ALU.mult),
                 reads=[PSS, PRM], writes=[cbm])
            S.op("dve", lambda e: e.tensor_tensor(out=Mm[:].rearrange("p (g r) l -> p g r l", g=2),
                                                  in0=Dm[:].rearrange("p (g r) l -> p g r l", g=2),
                                                  in1=cbm[:].unsqueeze(2).to_broadcast([128, 2, 4, 128]), op=ALU.mult),
                 reads=[Dm, cbm], writes=[Mm])
            for g in range(2):
                S.op("act", lambda e, g=g: e.activation(out=Ecs[g * 64:(g + 1) * 64, :, :],
                                                        in_=PSW[g * 64:(g + 1) * 64, g * 512:(g + 1) * 512].rearrange("p (r l) -> p r l", r=4),
                                                        func=AF.Exp), reads=[PSW], writes=[Ecs])
                S.op("dve", lambda e, g=g, sl=sl: e.tensor_tensor(out=CTs[g * 64:(g + 1) * 64, :, :], in0=Ecs[g * 64:(g + 1) * 64, :, :],
                                                                  in1=BCT[g * 64:(g + 1) * 64, 1, sl].unsqueeze(1).to_broadcast([64, 4, 128]),
                                                                  op=ALU.mult), reads=[Ecs, BCT], writes=[CTs])
            yield
            S.op("dve", lambda e, s=s: e.tensor_tensor(out=xdt[:].rearrange("p (h q) -> p h q", h=8),
                                                       in0=PST[:, 0:512].rearrange("p (h q) -> p h q", h=8),
                                                       in1=dts[:, s, :].unsqueeze(2).to_broadcast([128, 8, 64]), op=ALU.mult),
                 reads=[PST, dts], writes=[xdt])
            S.op("dve", lambda e: e.tensor_tensor(out=xdts[:].rearrange("p (h q) -> p h q", h=8),
                                                  in0=PST[:, 0:512].rearrange("p (h q) -> p h q", h=8),
                                                  in1=css[:, 16:24].unsqueeze(2).to_broadcast([128, 8, 64]), op=ALU.mult),
                 reads=[PST, css], writes=[xdts])
            S.op("act", lambda e: e.copy(out=Btok[:], in_=PST[:, 512:640]), reads=[PST], writes=[Btok])
            yield
            for h in range(8):
                g = h // 4
                r = h % 4
                osl = slice((h % 2) * 64, (h % 2) * 64 + 64)
                S.op("pe", lambda e, h=h, osl=osl: e.matmul(PSY[osl, h // 2, :], lhsT=xdt[:, h * 64:(h + 1) * 64], rhs=Mm[:, h, :],
                                                            start=True, stop=False), reads=[xdt, Mm], writes=[PSY])
                S.op("pe", lambda e, h=h, g=g, r=r, osl=osl: e.matmul(PSY[osl, h // 2, :], lhsT=sprev[:, h * 64:(h + 1) * 64],
                                                                      rhs=CTs[:, r, :], start=False, stop=True),
                     reads=[sprev, CTs], writes=[PSY])
            for j in range(4):
                o = offs["dsk"][0] + j
                S.op("dve", lambda e, j=j, o=o, sl=sl: e.scalar_tensor_tensor(out=yT[:, j, sl], in0=xsT[:, j, sl], scalar=PRM[:, o:o + 1],
                                                                              in1=PSY[:, j, :], op0=ALU.mult, op1=ALU.add),
                     reads=[xsT, PRM, PSY], writes=[yT])
            yield
            S.op("pe", lambda e: e.matmul(PSN[:, :], lhsT=Btok[:], rhs=xdts[:], start=True, stop=True), reads=[Btok, xdts], writes=[PSN])
            S.op("dve", lambda e: e.tensor_tensor(out=Sst[:].rearrange("p (h q) -> p h q", h=8), in0=Sst[:].rearrange("p (h q) -> p h q", h=8),
                                                  in1=css[:, 24:32].unsqueeze(2).to_broadcast([128, 8, 64]), op=ALU.mult),
                 reads=[Sst, css], writes=[Sst])
            S.op("dve", lambda e: e.tensor_tensor(out=Sst[:], in0=Sst[:], in1=PSN[:, :], op=ALU.add), reads=[Sst, PSN], writes=[Sst])
            for g in range(2):
                S.op("act", lambda e, snext=snext, g=g: e.copy(out=snext[g * 64:(g + 1) * 64, g * 256:(g + 1) * 256],
                                                               in_=Sst[g * 64:(g + 1) * 64, g * 256:(g + 1) * 256]), reads=[Sst], writes=[snext])
        if _os.environ.get("K_TRACE"): print("MARK", st, "gate", S.nrec)
        for j in range(4):
            yield
            S.op("dve", lambda e, j=j: e.tensor_tensor(out=yT[:, j, :], in0=yT[:, j, :], in1=zs[:, j, :], op=ALU.mult),
                 reads=[yT, zs], writes=[yT])
            S.op("act", lambda e, j=j: e.activation(out=xsTb[:, j, :], in_=yT[:, j, :], func=AF.Square), reads=[yT], writes=[xsTb])
        for g in range(2):
            yield
            ps = nextps()
            for jj in range(2):
                S.op("pe", lambda e, g=g, jj=jj, ps=ps: e.matmul(ps[:, 0:TS], lhsT=onesb[:], rhs=xsTb[:, 2 * g + jj, :],
                                                                 start=(jj == 0), stop=(jj == 1)), reads=[onesb, xsTb], writes=[ps])
            S.op("dve", lambda e, ps=ps: e.tensor_scalar(out=tmpA[:], in0=ps[:, 0:TS], scalar1=1.0 / 256, scalar2=EPS,
                                                         op0=ALU.mult, op1=ALU.add), reads=[ps], writes=[tmpA])
            S.op("act", lambda e: e.activation(out=tmpA[:], in_=tmpA[:], func=AF.Ln), reads=[tmpA], writes=[tmpA])
            S.op("act", lambda e: e.activation(out=tmpA[:], in_=tmpA[:], func=AF.Exp, scale=-0.5), reads=[tmpA], writes=[tmpA])
            for jj in range(2):
                j = 2 * g + jj
                o = offs["ggT"][0] + j
                S.op("dve", lambda e, j=j, o=o: e.scalar_tensor_tensor(out=GB[:, j, :], in0=yT[:, j, :], scalar=PRM[:, o:o + 1],
                                                                       in1=tmpA[:], op0=ALU.mult, op1=ALU.mult),
                     reads=[yT, PRM, tmpA], writes=[GB])
        if _os.environ.get("K_TRACE"): print("MARK", st, "outproj", S.nrec)
        for s in range(NS):
            yield
            proj_A(W_out, GB, s)
            S.op("dve", lambda e, s=s: e.tensor_tensor(out=xt[:, s, :], in0=xt[:, s, :], in1=PSW[:, :], op=ALU.add),
                 reads=[xt, PSW], writes=[xt])
            if dbg:
                S.dma("sp", None, dbg_h1[st * TS + s * 128:st * TS + (s + 1) * 128, :], xt, xt[:, s, :])

    def gen_X(st):
        xt = XT[st % 2]
        if _os.environ.get("K_TRACE"): print("MARK", st, "attn", S.nrec)
        norm_T(xt, "g2T", GA2, xn=xn2, ss=ss2, rs=rs2)
        for c in range(8):
            yield
            ps = nextps()
            proj_B(W_q, c * 128, GA2, ps)
            S.op("act", lambda e, c=c, ps=ps: e.activation(out=GC[:, c, :], in_=ps[:, 0:TS], func=AF.Copy, scale=1.0 / 16),
                 reads=[ps], writes=[GC])
        for s in range(NS):
            yield
            sl = slice(s * 128, (s + 1) * 128)
            for h in range(4):
                for kk in range(2):
                    S.op("pe", lambda e, h=h, kk=kk, sl=sl: e.matmul(PSW[:, h * MEM:(h + 1) * MEM], lhsT=GC[:, 2 * h + kk, sl],
                                                                     rhs=KT[:, 2 * h + kk, :], start=(kk == 0), stop=(kk == 1)),
                         reads=[GC, KT], writes=[PSW])
            S.op("dve", lambda e: e.tensor_reduce(out=mx[:, 0:4], in_=PSW[:, :].rearrange("p (h m) -> p h m", h=4), axis=AX.X, op=ALU.max),
                 reads=[PSW], writes=[mx])
            S.op("dve", lambda e: e.tensor_scalar(out=mx[:, 0:4], in0=mx[:, 0:4], scalar1=-1.0, scalar2=None, op0=ALU.mult),
                 reads=[mx], writes=[mx])
            for h in range(4):
                S.op("act", lambda e, h=h: e.activation(out=Pf[:, h, :], in_=PSW[:, h * MEM:(h + 1) * MEM], func=AF.Exp,
                                                        bias=mx[:, h:h + 1], scale=1.0, accum_out=mx[:, 4 + h:5 + h]),
                     reads=[PSW, mx], writes=[Pf, mx])
            S.op("dve", lambda e: e.reciprocal(out=mx[:, 4:8], in_=mx[:, 4:8]), reads=[mx], writes=[mx])
            S.op("dve", lambda e: e.tensor_tensor(out=Pn[:], in0=Pf[:], in1=mx[:, 4:8].unsqueeze(2).to_broadcast([128, 4, MEM]), op=ALU.mult),
                 reads=[Pf, mx], writes=[Pn])
            for c in range(8):
                S.op("pe", lambda e, c=c: e.transpose(out=PST[:, c * 128:(c + 1) * 128], in_=Pn[:, c // 2, (c % 2) * 128:(c % 2 + 1) * 128],
                                                      identity=identb[:]), reads=[Pn, identb], writes=[PST])
            S.op("act", lambda e, sl=sl: e.copy(out=GB2[:, :, sl], in_=PST[:].rearrange("p (c t) -> p c t", c=8)), reads=[PST], writes=[GB2])
        for c in range(8):
            yield
            ps = nextps()
            h = c // 2
            for mc in range(2):
                S.op("pe", lambda e, c=c, h=h, mc=mc, ps=ps: e.matmul(ps[:, 0:TS], lhsT=V[:, mc, c * 128:(c + 1) * 128], rhs=GB2[:, 2 * h + mc, :],
                                                                      start=(mc == 0), stop=(mc == 1)), reads=[V, GB2], writes=[ps])
            S.op("act", lambda e, c=c, ps=ps: e.copy(out=GA2[:, c, :], in_=ps[:, 0:TS]), reads=[ps], writes=[GA2])
        for s in range(NS):
            yield
            proj_A(W_o, GA2, s)
            S.op("dve", lambda e, s=s: e.tensor_tensor(out=xt[:, s, :], in0=xt[:, s, :], in1=PSW[:, :], op=ALU.add),
                 reads=[xt, PSW], writes=[xt])
            tok0 = st * TS + s * 128
            S.dma("sp", h2buf, h2buf.ap[tok0:tok0 + 128, :], xt, xt[:, s, :])
            if dbg:
                S.dma("sp", None, dbg_h2[tok0:tok0 + 128, :], xt, xt[:, s, :])
        if _os.environ.get("K_TRACE"): print("MARK", st, "route", S.nrec)
        for s in range(NS if stop != "A0" else 0):
            yield
            S.op("act", lambda e, s=s: e.activation(out=xn2[:], in_=xt[:, s, :], func=AF.Square, accum_out=ss2[:, s:s + 1]),
                 reads=[xt], writes=[xn2, ss2])
        rstd_from_ss(ss2, rs2, D, NS)
        for s in range(NS if stop != "A0" else 0):
            yield
            tile_i = st * NS + s
            tok0 = tile_i * 128
            S.op("dve", lambda e, s=s: e.scalar_tensor_tensor(out=n3[:], in0=xt[:, s, :], scalar=rs2[:, s:s + 1], in1=G3[:],
                                                              op0=ALU.mult, op1=ALU.mult), reads=[xt, rs2, G3], writes=[n3])
            S.op("act", lambda e: e.copy(out=n3b[:], in_=n3[:]), reads=[n3], writes=[n3b])
            S.dma("sp", n3buf, n3buf.ap[tok0:tok0 + 128, :], n3b, n3b[:])
            yield
            S.op("dve", lambda e: e.tensor_tensor(out=xn2[:], in0=n3[:], in1=n3b[:], op=ALU.subtract), reads=[n3, n3b], writes=[xn2])
            for src, dstT in ((n3b, GA2), (xn2, GC)):
                for k in range(8):
                    S.op("pe", lambda e, k=k, src=src: e.transpose(out=PST[:, k * 128:(k + 1) * 128], in_=src[:, k * 128:(k + 1) * 128], identity=identb[:]),
                         reads=[src, identb], writes=[PST])
                S.op("act", lambda e, dstT=dstT: e.copy(out=dstT[:, :, 0:128], in_=PST[:].rearrange("p (k t) -> p k t", k=8)), reads=[PST], writes=[dstT])
            combos = [(GA2, W_rh), (GC, W_rh), (GA2, W_rl)]
            for ci, (aT, wr) in enumerate(combos):
                for k in range(8):
                    S.op("pe", lambda e, k=k, aT=aT, wr=wr, ci=ci: e.matmul(PSS[:, RO:RO + 36], lhsT=aT[:, k, 0:128], rhs=wr[:, k, :],
                                                                            start=(ci == 0 and k == 0), stop=(ci == 2 and k == 7)),
                         reads=[aT, wr], writes=[PSS])
            S.op("dve", lambda e: e.tensor_tensor(out=L[:], in0=PSS[:, RO:RO + 36], in1=P("br"), op=ALU.add), reads=[PSS, PRM], writes=[L])
            S.op("dve", lambda e: e.tensor_reduce(out=rt[:, 0:1], in_=L[:, 0:4], axis=AX.X, op=ALU.max), reads=[L], writes=[rt])
            S.op("dve", lambda e: e.tensor_scalar(out=rt[:, 1:2], in0=rt[:, 0:1], scalar1=-1.0, scalar2=None, op0=ALU.mult), reads=[rt], writes=[rt])
            S.op("dve", lambda e: e.tensor_scalar(out=rt[:, 4:8], in0=L[:, 0:4], scalar1=rt[:, 0:1], scalar2=None, op0=ALU.is_equal),
                 reads=[L, rt], writes=[rt])
            S.op("act", lambda e: e.activation(out=rt[:, 32:36], in_=L[:, 0:4], func=AF.Exp, bias=rt[:, 1:2], scale=1.0, accum_out=rt[:, 2:3]),
                 reads=[L, rt], writes=[rt])
            S.op("dve", lambda e: e.reciprocal(out=rt[:, 3:4], in_=rt[:, 2:3]), reads=[rt], writes=[rt])
            S.op("dve", lambda e: e.tensor_scalar(out=rt[:, 8:12], in0=rt[:, 4:8], scalar1=-1.0, scalar2=1e9, op0=ALU.add, op1=ALU.mult),
                 reads=[rt], writes=[rt])
            S.op("dve", lambda e: e.tensor_tensor(out=rt[:, 64:96].rearrange("p (g j) -> p g j", g=4), in0=L[:, 4:36].rearrange("p (g j) -> p g j", g=4),
                                                  in1=rt[:, 8:12].unsqueeze(2).to_broadcast([128, 4, 8]), op=ALU.add), reads=[L, rt], writes=[rt])
            S.op("dve", lambda e: e.max(out=rt[:, 12:20], in_=rt[:, 64:96]), reads=[rt], writes=[rt])
            S.op("dve", lambda e: e.tensor_scalar(out=A0[:], in0=rt[:, 64:96], scalar1=rt[:, 12:13], scalar2=None, op0=ALU.is_equal),
                 reads=[rt], writes=[A0])
            S.op("dve", lambda e: e.tensor_scalar(out=A1[:], in0=rt[:, 64:96], scalar1=rt[:, 13:14], scalar2=None, op0=ALU.is_equal),
                 reads=[rt], writes=[A1])
            S.op("dve", lambda e: e.tensor_tensor(out=Ab[:], in0=A0[:], in1=A1[:], op=ALU.add), reads=[A0, A1], writes=[Ab])
            yield
            S.op("dve", lambda e: e.tensor_tensor(out=rt[:, 20:21], in0=rt[:, 13:14], in1=rt[:, 12:13], op=ALU.subtract), reads=[rt], writes=[rt])
            S.op("act", lambda e: e.activation(out=rt[:, 21:22], in_=rt[:, 20:21], func=AF.Exp), reads=[rt], writes=[rt])
            S.op("dve", lambda e: e.tensor_scalar(out=rt[:, 22:23], in0=rt[:, 21:22], scalar1=1.0, scalar2=None, op0=ALU.add), reads=[rt], writes=[rt])
            S.op("dve", lambda e: e.reciprocal(out=rt[:, 22:23], in_=rt[:, 22:23]), reads=[rt], writes=[rt])
            S.op("dve", lambda e: e.tensor_tensor(out=rt[:, 23:24], in0=rt[:, 21:22], in1=rt[:, 22:23], op=ALU.mult), reads=[rt], writes=[rt])
            S.op("dve", lambda e: e.tensor_scalar(out=rt[:, 24:26], in0=rt[:, 22:24], scalar1=rt[:, 3:4], scalar2=None, op0=ALU.mult),
                 reads=[rt], writes=[rt])
            yield
            S.op("pe", lambda e: e.matmul(PSS[:, RO + 36:RO + 68], lhsT=trisb[:], rhs=Ab[:], start=True, stop=True), reads=[trisb, Ab], writes=[PSS])
            S.op("pe", lambda e: e.matmul(PSS[:, RO + 68:RO + 100], lhsT=onesb[:], rhs=Ab[:], start=True, stop=True), reads=[onesb, Ab], writes=[PSS])
            S.op("dve", lambda e: e.tensor_tensor(out=posf[:], in0=PSS[:, RO + 36:RO + 68], in1=cnt[:], op=ALU.add), reads=[PSS, cnt], writes=[posf])
            S.op("dve", lambda e: e.tensor_tensor(out=cnt[:], in0=cnt[:], in1=PSS[:, RO + 68:RO + 100], op=ALU.add), reads=[PSS, cnt], writes=[cnt])
            for k, Ak in enumerate((A0, A1)):
                S.op("dve", lambda e, Ak=Ak: e.tensor_tensor(out=rt[:, 96:128], in0=Ak[:], in1=posf[:], op=ALU.mult), reads=[Ak, posf], writes=[rt])
                S.op("dve", lambda e, k=k: e.tensor_reduce(out=rt[:, 26 + k:27 + k], in_=rt[:, 96:128], axis=AX.X, op=ALU.add), reads=[rt], writes=[rt])
                S.op("dve", lambda e, Ak=Ak: e.tensor_tensor(out=rt[:, 96:128], in0=Ak[:], in1=P("eidx"), op=ALU.mult), reads=[Ak, PRM], writes=[rt])
                S.op("dve", lambda e, k=k: e.tensor_reduce(out=rt[:, 28 + k:29 + k], in_=rt[:, 96:128], axis=AX.X, op=ALU.add), reads=[rt], writes=[rt])
            S.op("dve", lambda e: e.tensor_scalar(out=rt[:, 30:32], in0=rt[:, 26:28], scalar1=float(CAP), scalar2=None, op0=ALU.is_lt), reads=[rt], writes=[rt])
            S.op("dve", lambda e: e.tensor_scalar(out=rt[:, 36:38], in0=rt[:, 30:32], scalar1=-1.0, scalar2=-1.0, op0=ALU.add, op1=ALU.mult),
                 reads=[rt], writes=[rt])
            yield
            S.op("dve", lambda e: e.tensor_copy(out=pki[:, 0:2], in_=rt[:, 26:28]), reads=[rt], writes=[pki])
            S.op("dve", lambda e: e.tensor_scalar(out=pki[:, 2:4], in0=pki[:, 0:2], scalar1=7, scalar2=None, op0=ALU.arith_shift_right),
                 reads=[pki], writes=[pki])
            S.op("dve", lambda e: e.tensor_scalar(out=pki[:, 4:6], in0=pki[:, 0:2], scalar1=127, scalar2=None, op0=ALU.bitwise_and),
                 reads=[pki], writes=[pki])
            S.op("dve", lambda e: e.tensor_copy(out=rt[:, 40:44], in_=pki[:, 2:6]), reads=[pki], writes=[rt])
            S.op("dve", lambda e: e.tensor_scalar(out=rt[:, 44:46], in0=rt[:, 42:44], scalar1=float(NB), scalar2=None, op0=ALU.mult), reads=[rt], writes=[rt])
            S.op("dve", lambda e: e.scalar_tensor_tensor(out=rt[:, 44:46], in0=rt[:, 28:30], scalar=float(CB), in1=rt[:, 44:46], op0=ALU.mult, op1=ALU.add),
                 reads=[rt], writes=[rt])
            S.op("dve", lambda e: e.tensor_tensor(out=rt[:, 44:46], in0=rt[:, 44:46], in1=rt[:, 40:42], op=ALU.add), reads=[rt], writes=[rt])
            S.op("dve", lambda e: e.tensor_tensor(out=rt[:, 44:46], in0=rt[:, 44:46], in1=rt[:, 30:32], op=ALU.mult), reads=[rt], writes=[rt])
            S.op("dve", lambda e: e.scalar_tensor_tensor(out=rt[:, 44:46], in0=rt[:, 36:38], scalar=DRV[:, 48:49], in1=rt[:, 44:46], op0=ALU.mult, op1=ALU.add),
                 reads=[rt, DRV], writes=[rt])
            S.op("dve", lambda e: e.scalar_tensor_tensor(out=rt[:, 46:48], in0=rt[:, 28:30], scalar=float(CAP), in1=rt[:, 26:28], op0=ALU.mult, op1=ALU.add),
                 reads=[rt], writes=[rt])
            S.op("dve", lambda e: e.tensor_tensor(out=rt[:, 46:48], in0=rt[:, 46:48], in1=rt[:, 30:32], op=ALU.mult), reads=[rt], writes=[rt])
            S.op("dve", lambda e: e.scalar_tensor_tensor(out=rt[:, 46:48], in0=rt[:, 36:38], scalar=float(NSLOT), in1=rt[:, 46:48], op0=ALU.mult, op1=ALU.add),
                 reads=[rt], writes=[rt])
            S.op("dve", lambda e, tile_i=tile_i: e.tensor_copy(out=ROWS[:, tile_i, :], in_=rt[:, 46:48]), reads=[rt], writes=[ROWS])
            yield
            S.op("dve", lambda e, tok0=tok0: e.tensor_scalar(out=rt[:, 48:49], in0=P("pidx"), scalar1=float(tok0), scalar2=None, op0=ALU.add),
                 reads=[PRM], writes=[rt])
            for k in range(2):
                en = ent[(tile_i * 2 + k) % 4]
                si = sidx[(tile_i * 2 + k) % 4]
                S.op("dve", lambda e, en=en: e.tensor_copy(out=en[:, 0:1], in_=rt[:, 48:49]), reads=[rt], writes=[en])
                S.op("dve", lambda e, en=en, k=k: e.tensor_copy(out=en[:, 1:2].bitcast(F32), in_=rt[:, 24 + k:25 + k]), reads=[rt], writes=[en])
                S.op("dve", lambda e, si=si, k=k: e.tensor_copy(out=si[:], in_=rt[:, 44 + k:45 + k]), reads=[rt], writes=[si])
                S.op("pool", lambda e, en=en, si=si: e.indirect_dma_start(out=tab.ap[:, :], out_offset=bass.IndirectOffsetOnAxis(ap=si[:, :], axis=0),
                                                                          in_=en[:, :], in_offset=None),
                     reads=[en, si, tab], dma=True)

        if st + 2 < NST:
            load_x(st + 2)
        yield

    load_x(0)
    if NST > 1:
        load_x(1)
    for _ in gen_M(0):
        pass
    for st in range(NST):
        gens = [gen_X(st)]
        if st + 1 < NST:
            gens.append(gen_M(st + 1))
        while gens:
            for g_ in list(gens):
                try:
                    for _ in range(BURST):
                        next(g_)
                except StopIteration:
                    gens.remove(g_)
    S.flush()
    S.pop()
    if stop in ("A", "A0"):
        S.pop()
        return nc, S

    S.push()
    TAB = S.sb([128, NB, 2], I32, "TAB")
    S.dma("sp", TAB, TAB[:].rearrange("p a b -> p (a b)"), tab, tab.ap[0:128 * NB, :].rearrange("(p a) b -> p (a b)", p=128))
    Wg = [S.sb([128, 8, DE], BF16, f"wg{i}") for i in range(2)]
    Wu = [S.sb([128, 8, DE], BF16, f"wu{i}") for i in range(2)]
    Wd = [S.sb([128, 4, D], BF16, f"wd{i}") for i in range(2)]
    xb = [S.sb([128, D], BF16, f"xb{i}") for i in range(2 * CB)]
    xbT = S.sb([128, 8, CAP], BF16, "xbT")
    hT = S.sb([128, 4, CAP], BF16, "hT")
    th = [S.sb([128, 512], F32, f"th{i}") for i in range(2)]
    t2 = [S.sb([128, 512], F32, f"t2{i}") for i in range(2)]
    yo = [S.sb([128, D], F32, f"yo{i}") for i in range(2)]
    PT2 = [S.ps([128, 1024], BF16, f"pt2{i}") for i in range(2)]
    PG = [S.ps([128, 512], F32, f"pg{i}") for i in range(2)]
    PU = [S.ps([128, 512], F32, f"pu{i}") for i in range(2)]
    PD = [S.ps([128, 512], F32, f"pd{i}") for i in range(2)]

    def load_w(e):
        i = e % 2
        S.dma("pool", Wg[i], Wg[i][:], None, w_g_d[e].rearrange("(c p) n -> p c n", p=128))
        S.dma("pool", Wu[i], Wu[i][:], None, w_u_d[e].rearrange("(c p) n -> p c n", p=128))
        S.dma("pool", Wd[i], Wd[i][:], None, w_d_d[e].rearrange("(c p) n -> p c n", p=128))

    halves = []
    o = 0
    while o < CAP:
        n = min(512, CAP - o)
        halves.append((o, n))
        o += n
    def gather_x(ex):
        for b in range(CB):
            blk = ex * CB + b
            xg = xb[blk % (2 * CB)]
            S.op("pool", lambda e, xg=xg, blk=blk: e.indirect_dma_start(out=xg[:, :], out_offset=None, in_=n3buf.ap[:, :],
                                                                        in_offset=bass.IndirectOffsetOnAxis(ap=TAB[:, blk, 0:1], axis=0)),
                 reads=[TAB, n3buf], writes=[xg], dma=True)

    gather_x(0)
    load_w(0)
    gi = 0
    for ex in range(NEXP):
        if ex + 1 < NEXP:
            gather_x(ex + 1)
            load_w(ex + 1)
        wi = ex % 2
        for b in range(CB):
            blk = ex * CB + b
            xg = xb[blk % (2 * CB)]
            pt = PT2[gi % 2]
            for k in range(8):
                S.op("pe", lambda e, k=k, xg=xg, pt=pt: e.transpose(out=pt[:, k * 128:(k + 1) * 128], in_=xg[:, k * 128:(k + 1) * 128], identity=identb[:]),
                     reads=[xg, identb], writes=[pt])
            eng = "act" if gi % 2 == 0 else "dve"
            if eng == "act":
                S.op("act", lambda e, pt=pt, b=b: e.copy(out=xbT[:, :, b * 128:(b + 1) * 128], in_=pt[:].rearrange("p (k t) -> p k t", k=8)),
                     reads=[pt], writes=[xbT])
            else:
                S.op("dve", lambda e, pt=pt, b=b: e.tensor_copy(out=xbT[:, :, b * 128:(b + 1) * 128], in_=pt[:].rearrange("p (k t) -> p k t", k=8)),
                     reads=[pt], writes=[xbT])
            gi += 1
        hi = 0
        for f in range(4):
            for (o0, n) in halves:
                pg = PG[hi % 2]
                pu = PU[hi % 2]
                tt = th[hi % 2]
                t22 = t2[hi % 2]
                hi += 1
                for k in range(8):
                    S.op("pe", lambda e, k=k, f=f, o0=o0, n=n, pg=pg: e.matmul(pg[:, 0:n], lhsT=Wg[wi][:, k, f * 128:(f + 1) * 128], rhs=xbT[:, k, o0:o0 + n],
                                                                               start=(k == 0), stop=(k == 7)), reads=[Wg[wi], xbT], writes=[pg])
                for k in range(8):
                    S.op("pe", lambda e, k=k, f=f, o0=o0, n=n, pu=pu: e.matmul(pu[:, 0:n], lhsT=Wu[wi][:, k, f * 128:(f + 1) * 128], rhs=xbT[:, k, o0:o0 + n],
                                                                               start=(k == 0), stop=(k == 7)), reads=[Wu[wi], xbT], writes=[pu])
                S.op("act", lambda e, n=n, pg=pg, tt=tt: e.activation(out=tt[:, 0:n], in_=pg[:, 0:n], func=AF.Tanh, scale=0.5), reads=[pg], writes=[tt])
                S.op("dve", lambda e, n=n, pg=pg, tt=tt, t22=t22: e.scalar_tensor_tensor(out=t22[:, 0:n], in0=tt[:, 0:n], scalar=1.0, in1=pg[:, 0:n],
                                                                                         op0=ALU.add, op1=ALU.mult), reads=[tt, pg], writes=[t22])
                S.op("dve", lambda e, n=n, pu=pu, t22=t22, f=f, o0=o0: e.scalar_tensor_tensor(out=hT[:, f, o0:o0 + n], in0=t22[:, 0:n], scalar=0.5, in1=pu[:, 0:n],
                                                                                              op0=ALU.mult, op1=ALU.mult), reads=[t22, pu], writes=[hT])
        for b in range(CB):
            blk = ex * CB + b
            y = yo[blk % 2]
            for hf in range(2):
                for f in range(4):
                    S.op("pe", lambda e, f=f, hf=hf, b=b: e.matmul(PD[hf][:, :], lhsT=hT[:, f, b * 128:(b + 1) * 128],
                                                                   rhs=Wd[wi][:, f, hf * 512:(hf + 1) * 512], start=(f == 0), stop=(f == 3)),
                         reads=[hT, Wd[wi]], writes=[PD[hf]])
                if hf == 0:
                    S.op("act", lambda e, y=y, blk=blk, hf=hf: e.activation(out=y[:, hf * 512:(hf + 1) * 512], in_=PD[hf][:, :], func=AF.Copy,
                                                                            scale=TAB[:, blk, 1:2].bitcast(F32)),
                         reads=[PD[hf], TAB], writes=[y])
                else:
                    S.op("dve", lambda e, y=y, blk=blk, hf=hf: e.tensor_scalar(out=y[:, hf * 512:(hf + 1) * 512], in0=PD[hf][:, :],
                                                                               scalar1=TAB[:, blk, 1:2].bitcast(F32), scalar2=None, op0=ALU.mult),
                         reads=[PD[hf], TAB], writes=[y])
            S.dma("sp", None, ybuf.ap[blk * 128:(blk + 1) * 128, :], y, y[:], extra_reads=[ybuf])
    S.flush()
    S.pop()
    if stop == "B":
        S.pop()
        return nc, S

    S.push()
    NBUF = 4
    hb = [S.sb([128, D], F32, f"hb{i}") for i in range(NBUF)]
    y0 = [S.sb([128, D], F32, f"y0{i}") for i in range(NBUF)]
    y1 = [S.sb([128, D], F32, f"y1{i}") for i in range(NBUF)]
    ob = [S.sb([128, D], F32, f"ob{i}") for i in range(NBUF)]
    jk = S.sb([128, D], BF16, "jk")
    GF = S.sb([128, D], F32, "gF")
    S.dma("sp", GF, GF[:], None, gF_d[:, :])
    ssc = [S.sb([128, 1], F32, f"ssc{i}") for i in range(NBUF)]
    def c_load(t):
        i = t % NBUF
        S.dma("sp", hb[i], hb[i][:], h2buf, h2buf.ap[t * 128:(t + 1) * 128, :])
        for k, yy in enumerate((y0[i], y1[i])):
            S.op("pool", lambda e, yy=yy, t=t, k=k: e.indirect_dma_start(out=yy[:, :], out_offset=None, in_=ybuf.ap[:, :],
                                                                         in_offset=bass.IndirectOffsetOnAxis(ap=ROWS[:, t, k:k + 1], axis=0)),
                 reads=[ROWS, ybuf], writes=[yy], dma=True)

    for t in range(min(NBUF - 1, NT)):
        c_load(t)
    for t in range(NT):
        i = t % NBUF
        if t + NBUF - 1 < NT:
            c_load(t + NBUF - 1)
        S.op("dve", lambda e, i=i: e.tensor_tensor(out=hb[i][:], in0=hb[i][:], in1=y0[i][:], op=ALU.add), reads=[hb[i], y0[i]], writes=[hb[i]])
        S.op("dve", lambda e, i=i: e.tensor_tensor(out=hb[i][:], in0=hb[i][:], in1=y1[i][:], op=ALU.add), reads=[hb[i], y1[i]], writes=[hb[i]])
        S.op("act", lambda e, i=i: e.activation(out=jk[:], in_=hb[i][:], func=AF.Square, accum_out=ssc[i][:, 0:1]), reads=[hb[i]], writes=[jk, ssc[i]])
        rstd_from_ss(ssc[i], ssc[i], D)
        S.op("dve", lambda e, i=i: e.scalar_tensor_tensor(out=ob[i][:], in0=hb[i][:], scalar=ssc[i][:, 0:1], in1=GF[:], op0=ALU.mult, op1=ALU.mult),
             reads=[hb[i], ssc[i], GF], writes=[ob[i]])
        S.dma("sp", None, out_d[t * 128:(t + 1) * 128, :], ob[i], ob[i][:])
    S.flush()
    S.pop()
    S.pop()
    return nc, S


_CACHE = {}


def make_in_maps(inputs, SEQ, CB):
    inp = {k: np.asarray(v) for k, v in inputs.items()}
    offs, prm = pack_params(inp, CB * 128)
    l = 0
    w_r = np.ascontiguousarray(np.concatenate([inp["w_router_group"][l], inp["w_router_expert"][l]], axis=1), dtype=np.float32)
    shared = {
        "prm": prm,
        "w_in": np.ascontiguousarray(inp["w_in"][l], dtype=np.float32),
        "w_out": np.ascontiguousarray(inp["w_out"][l], dtype=np.float32),
        "w_q": np.ascontiguousarray(inp["w_q"][l], dtype=np.float32),
        "w_kv": np.ascontiguousarray(inp["w_kv"][l], dtype=np.float32),
        "w_o": np.ascontiguousarray(inp["w_o"][l], dtype=np.float32),
        "w_r": w_r,
        "w_gate": np.ascontiguousarray(inp["w_gate"][l], dtype=np.float32),
        "w_up": np.ascontiguousarray(inp["w_up"][l], dtype=np.float32),
        "w_down": np.ascontiguousarray(inp["w_down"][l], dtype=np.float32),
        "g3b": np.ascontiguousarray(_bc(inp["norm_moe"][l])),
        "gFb": np.ascontiguousarray(_bc(inp["norm_final"])),
    }
    B = inp["x"].shape[0]
    maps = []
    for b in range(B):
        m = dict(shared)
        m["x"] = np.ascontiguousarray(inp["x"][b], dtype=np.float32)
        m["mem"] = np.ascontiguousarray(inp["mem"][b], dtype=np.float32)
        maps.append(m)
    return offs, prm.shape[1], maps


def kernel(**inputs):
    SEQ = int(np.asarray(inputs["x"]).shape[1])
    TS = 256
    CB = max(1, (SEQ * 2 // NEXP) * 3 // 2 // 128)
    offs, nprm, maps = make_in_maps(inputs, SEQ, CB)
    key = (SEQ, TS, CB)
    if key not in _CACHE:
        _CACHE[key] = build(SEQ, TS, CB, offs, nprm)[0]
    nc = _CACHE[key]
    res = run_bass_kernel_spmd(nc, maps, core_ids=list(range(len(maps))))
    out = np.stack([np.asarray(r["out"], dtype=np.float32) for r in res.results], axis=0)
    return out
```

```python
import numpy as np
import concourse.bass as bass
import concourse.mybir as mybir
from concourse.bass_utils import run_bass_kernel_spmd

F32 = mybir.dt.float32
BF16 = mybir.dt.bfloat16
I32 = mybir.dt.int32
ALU = mybir.AluOpType
AF = mybir.ActivationFunctionType
AX = mybir.AxisListType

D = 1024
MEM = 256
NEXP = 32
DE = 512
IN_COLS = 2824
EPS = 1e-6


class T:
    __slots__ = ("ap", "name", "w", "r", "rd", "ep", "root")

    def __init__(self, ap, name="", base=None):
        self.ap = ap
        self.name = name
        self.w = None
        self.r = {}
        self.rd = []
        self.ep = -1
        self.root = base.root if base is not None else self

    def __getitem__(self, k):
        return self.ap[k]


class Op:
    __slots__ = ("eng", "fn", "dma", "deps", "sig", "sigval", "dsem", "dval", "prev", "ep")

    def __init__(self, eng, fn, dma, ep):
        self.eng = eng
        self.fn = fn
        self.dma = dma
        self.deps = []
        self.sig = False
        self.sigval = 0
        self.dsem = None
        self.dval = 0
        self.prev = None
        self.ep = ep


class _Rec:
    def __init__(self):
        self.call = None

    def __getattr__(self, name):
        def f(*a, **k):
            self.call = (name, a, k)
            return None
        return f


class Sched:
    DMA_POOL = {"sp": 24, "act": 4, "pool": 16}

    def __init__(self, nc):
        self.nc = nc
        self.ops = []
        self.engs = {"pe": nc.tensor, "act": nc.scalar, "dve": nc.vector,
                     "pool": nc.gpsimd, "sp": nc.sync}
        self._n = 0
        self.ep = 0
        self.esem = {k: nc.alloc_semaphore(f"s_{k}") for k in ("pe", "act", "dve", "pool")}
        self.pools = {q: [nc.alloc_semaphore(f"d_{q}{i}") for i in range(n)]
                      for q, n in self.DMA_POOL.items()}
        self.cnt = {k: 0 for k in self.esem}
        self.dcnt = {q: 0 for q in self.pools}
        self.hist = {q: [] for q in self.pools}
        self.waited = {k: {} for k in self.engs}
        self.scopes = []
        self.nwait = 0
        self.nops = 0
        self.maxops = None
        self.nrec = 0

    def push(self):
        self.scopes.append([])

    def pop(self):
        for g in reversed(self.scopes.pop()):
            g.__exit__(None, None, None)

    def sb(self, shape, dtype, name="sb"):
        self._n += 1
        g = self.nc.sbuf_tensor(f"{name}_{self._n}", list(shape), dtype)
        h = g.__enter__()
        self.scopes[-1].append(g)
        return T(h.ap(), name)

    def ps(self, shape, dtype, name="ps"):
        self._n += 1
        g = self.nc.psum_tensor(f"{name}_{self._n}", list(shape), dtype)
        h = g.__enter__()
        self.scopes[-1].append(g)
        return T(h.ap(), name)

    def dram(self, shape, dtype, name):
        return T(self.nc.dram_tensor(name, list(shape), dtype).ap(), name)

    def _chk(self, t):
        if t.ep != self.ep:
            t.w = None
            t.r = {}
            t.rd = []
            t.ep = self.ep

    def op(self, eng, fn, reads=(), writes=(), dma=False):
        self.nrec += 1
        if self.maxops is not None and self.nrec > self.maxops:
            return None
        rec = _Rec()
        fn(rec)
        assert rec.call is not None
        o = Op(eng, rec.call, dma, self.ep)
        reads = [t.root for t in reads]
        writes = [t.root for t in writes]
        raw = []
        war = []
        for t in reads:
            self._chk(t)
            if t.w is not None:
                raw.append(t.w)
        for t in writes:
            self._chk(t)
            if t.w is not None:
                raw.append(t.w)
            war.extend(t.r.values())
            war.extend(t.rd)
        deps = []
        seen = set()
        for d in raw:
            if id(d) in seen:
                continue
            seen.add(id(d))
            if (not d.dma) and (not dma) and d.eng == eng and eng == "pe":
                continue
            deps.append(d)
        for d in war:
            if id(d) in seen:
                continue
            seen.add(id(d))
            if (not d.dma) and (not dma) and d.eng == eng and eng == "pe":
                continue
            deps.append(d)
        o.deps = deps
        for t in reads:
            if dma:
                t.rd.append(o)
            else:
                t.r[eng] = o
        for t in writes:
            t.w = o
            t.r = {}
            t.rd = []
        self.ops.append(o)
        return o

    def dma(self, q, out_t, out_ap, in_t, in_ap, extra_reads=(), **kw):
        reads = [t for t in (in_t,) if t is not None] + list(extra_reads)
        writes = [t for t in (out_t,) if t is not None]
        return self.op(q, lambda e: e.dma_start(out=out_ap, in_=in_ap, **kw),
                       reads=reads, writes=writes, dma=True)

    def flush(self):
        ops = self.ops
        self.ops = []
        last = {}
        for o in ops:
            for d in o.deps:
                d.sig = True
            if not o.dma:
                last[o.eng] = o
        for o in last.values():
            o.sig = True
        for o in ops:
            if o.dma:
                K = len(self.pools[o.eng])
                i = self.dcnt[o.eng]
                o.dsem = self.pools[o.eng][i % K]
                o.dval = 16 * (i // K + 1)
                o.prev = self.hist[o.eng][i - K] if i >= K else None
                self.hist[o.eng].append(o)
                self.dcnt[o.eng] = i + 1
            elif o.sig:
                self.cnt[o.eng] += 1
                o.sigval = self.cnt[o.eng]
        for o in ops:
            e = self.engs[o.eng]
            needs = []
            for d in o.deps:
                if d.dma:
                    needs.append((d.dsem, d.dval))
                else:
                    needs.append((self.esem[d.eng], d.sigval))
            if o.dma and o.prev is not None:
                needs.append((o.prev.dsem, o.prev.dval))
            w = self.waited[o.eng]
            for sem, val in needs:
                key = id(sem)
                if w.get(key, 0) < val:
                    e.wait_ge(sem, val)
                    w[key] = val
                    self.nwait += 1
            name, a, k = o.fn
            inst = getattr(e, name)(*a, **k)
            if o.dma:
                inst.then_inc(o.dsem, 16)
            elif o.sig:
                inst.then_inc(self.esem[o.eng], 1)
        self.nops += len(ops)
        targets = [(self.esem[k], self.cnt[k]) for k in self.esem if self.cnt[k] > 0]
        for q, lst in self.hist.items():
            lastv = {}
            for o in lst[-len(self.pools[q]):]:
                lastv[id(o.dsem)] = (o.dsem, o.dval)
            targets.extend(lastv.values())
        for k, e in self.engs.items():
            w = self.waited[k]
            for sem, val in targets:
                if k in self.esem and sem is self.esem[k]:
                    continue
                if w.get(id(sem), 0) < val:
                    e.wait_ge(sem, val)
                    w[id(sem)] = val
        self.ep += 1


def _pack(items):
    offs = {}
    cols = []
    o = 0
    for name, arr in items:
        arr = np.ascontiguousarray(arr, dtype=np.float32).reshape(128, -1)
        offs[name] = (o, arr.shape[1])
        cols.append(arr)
        o += arr.shape[1]
    return offs, np.ascontiguousarray(np.concatenate(cols, axis=1))


def _bc(v):
    v = np.asarray(v, dtype=np.float32).reshape(-1)
    return np.broadcast_to(v[None, :], (128, v.shape[0]))


def _colT(v, nchunk):
    return np.asarray(v, dtype=np.float32).reshape(nchunk, 128).T


def pack_params(inp, CAP):
    l = 0
    items = [
        ("g1T", _colT(inp["norm_mix"][l], 8)),
        ("g2T", _colT(inp["norm_xattn"][l], 8)),
        ("gmT", _colT(inp["norm_mem"], 8)),
        ("ggT", _colT(inp["norm_ssd_gate"][l], 4)),
        ("cw", np.asarray(inp["conv_ssd_w"][l]).T.reshape(6, 128, 4).transpose(1, 0, 2)),
        ("cb", _colT(inp["conv_ssd_b"][l], 6)),
        ("sw", np.asarray(inp["conv_short_w"][l]).T.reshape(4, 128, 3).transpose(1, 0, 2)),
        ("dsk", _colT(np.repeat(np.asarray(inp["d_skip"][l]), 64), 4)),
        ("dtb", _bc(inp["dt_bias"][l])),
        ("alog", _bc(inp["a_log"][l])),
        ("br", _bc(np.concatenate([np.asarray(inp["b_router_group"][l]), np.asarray(inp["b_router_expert"][l])]))),
        ("ident", np.eye(128, dtype=np.float32)),
        ("tri", np.triu(np.ones((128, 128), np.float32))),
        ("tris", np.triu(np.ones((128, 128), np.float32), 1)),
        ("ones", np.ones((128, 128), np.float32)),
        ("eidx", _bc(np.arange(32))),
        ("pidx", np.arange(128, dtype=np.float32).reshape(128, 1)),
    ]
    return _pack(items)


def build(SEQ, TS, CB, offs, NPRM, dbg=False, stop=None, BURST=1):
    NS = TS // 128
    NST = SEQ // TS
    NT = SEQ // 128
    CAP = CB * 128
    NSLOT = NEXP * CAP
    NB = NEXP * CB

    nc = bass.Bass("TRN2", target_bir_lowering=False)
    S = Sched(nc)
    import os as _os
    if _os.environ.get("K_MAXOPS"):
        S.maxops = int(_os.environ["K_MAXOPS"])

    def din(name, shape, dt=F32):
        return nc.dram_tensor(name, list(shape), dt, kind="ExternalInput").ap()

    x_d = din("x", [SEQ, D])
    mem_d = din("mem", [MEM, D])
    prm_d = din("prm", [128, NPRM])
    w_in_d = din("w_in", [D, IN_COLS])
    w_out_d = din("w_out", [D, D])
    w_q_d = din("w_q", [D, D])
    w_kv_d = din("w_kv", [D, 2 * D])
    w_o_d = din("w_o", [D, D])
    w_r_d = din("w_r", [D, 36])
    w_g_d = din("w_gate", [NEXP, D, DE])
    w_u_d = din("w_up", [NEXP, D, DE])
    w_d_d = din("w_down", [NEXP, DE, D])
    g3_d = din("g3b", [128, D])
    gF_d = din("gFb", [128, D])
    out_d = nc.dram_tensor("out", [SEQ, D], F32, kind="ExternalOutput").ap()
    if dbg:
        dbg_h1 = nc.dram_tensor("dbg_h1", [SEQ, D], F32, kind="ExternalOutput").ap()
        dbg_h2 = nc.dram_tensor("dbg_h2", [SEQ, D], F32, kind="ExternalOutput").ap()

    h2buf = S.dram([SEQ, D], F32, "h2buf")
    n3buf = S.dram([SEQ, D], BF16, "n3buf")
    ybuf = S.dram([NSLOT + 128, D], F32, "ybuf")
    tab = S.dram([128 * NB + 128, 2], I32, "tab")

    S.push()
    PRM = S.sb([128, NPRM], F32, "prm")

    def P(name, a=None, b=None):
        o, n = offs[name]
        if a is None:
            return PRM[:, o:o + n]
        return PRM[:, o + a:o + b]

    ROWS = S.sb([128, NT, 2], I32, "rows")
    identb = S.sb([128, 128], BF16, "identb")
    onesb = S.sb([128, 128], BF16, "onesb")
    trisb = S.sb([128, 128], BF16, "trisb")
    trib = S.sb([128, 128], BF16, "trib")

    S.dma("sp", PRM, PRM[:], None, prm_d[:, :])
    S.op("dve", lambda e: e.tensor_copy(out=identb[:], in_=P("ident")), reads=[PRM], writes=[identb])
    S.op("dve", lambda e: e.tensor_copy(out=onesb[:], in_=P("ones")), reads=[PRM], writes=[onesb])
    S.op("dve", lambda e: e.tensor_copy(out=trisb[:], in_=P("tris")), reads=[PRM], writes=[trisb])
    S.op("dve", lambda e: e.tensor_copy(out=trib[:], in_=P("tri")), reads=[PRM], writes=[trib])

    def rstd_from_ss(ss, out, n, width=1):
        S.op("dve", lambda e: e.tensor_scalar(out=out[:, 0:width], in0=ss[:, 0:width], scalar1=1.0 / n, scalar2=EPS,
                                              op0=ALU.mult, op1=ALU.add), reads=[ss], writes=[out])
        S.op("act", lambda e: e.activation(out=out[:, 0:width], in_=out[:, 0:width], func=AF.Ln), reads=[out], writes=[out])
        S.op("act", lambda e: e.activation(out=out[:, 0:width], in_=out[:, 0:width], func=AF.Exp, scale=-0.5), reads=[out], writes=[out])

    S.push()
    W_in = S.sb([128, 8, IN_COLS], BF16, "w_in")
    W_out = S.sb([128, 8, D], BF16, "w_out")
    W_q = S.sb([128, 8, D], BF16, "w_q")
    W_o = S.sb([128, 8, D], BF16, "w_o")
    W_r = S.sb([128, 8, 36], F32, "w_r")
    KT = S.sb([128, 8, MEM], BF16, "KT")
    V = S.sb([128, 2, D], BF16, "V")
    G3 = S.sb([128, D], F32, "g3")
    DRV = S.sb([128, 64], F32, "drv")
    S.dma("pool", W_in, W_in[:], None, w_in_d.rearrange("(c p) n -> p c n", p=128))
    S.dma("sp", W_r, W_r[:], None, w_r_d.rearrange("(c p) n -> p c n", p=128))
    W_rh = S.sb([128, 8, 36], BF16, "w_rh")
    W_rl = S.sb([128, 8, 36], BF16, "w_rl")
    S.op("dve", lambda e: e.tensor_copy(out=W_rh[:], in_=W_r[:]), reads=[W_r], writes=[W_rh])
    S.op("dve", lambda e: e.tensor_tensor(out=W_rl[:], in0=W_r[:], in1=W_rh[:], op=ALU.subtract), reads=[W_r, W_rh], writes=[W_rl])
    S.dma("sp", G3, G3[:], None, g3_d[:, :])
    S.op("dve", lambda e: e.tensor_scalar(out=DRV[:, 0:24], in0=P("cw"), scalar1=0.5, scalar2=None, op0=ALU.mult),
         reads=[PRM], writes=[DRV])
    S.op("dve", lambda e: e.tensor_scalar(out=DRV[:, 24:30], in0=P("cb"), scalar1=0.5, scalar2=None, op0=ALU.mult),
         reads=[PRM], writes=[DRV])
    S.op("act", lambda e: e.activation(out=DRV[:, 32:40], in_=P("alog"), func=AF.Exp), reads=[PRM], writes=[DRV])
    S.op("dve", lambda e: e.tensor_scalar(out=DRV[:, 48:49], in0=P("pidx"), scalar1=float(128 * NB), scalar2=None, op0=ALU.add),
         reads=[PRM], writes=[DRV])
    S.op("dve", lambda e: e.tensor_scalar(out=DRV[:, 32:40], in0=DRV[:, 32:40], scalar1=-1.0, scalar2=None, op0=ALU.mult),
         reads=[DRV], writes=[DRV])

    S.push()
    W_kv = S.sb([128, 8, 2 * D], BF16, "w_kv")
    mt = S.sb([128, 2, D], F32, "mt")
    mnT = S.sb([128, 8, MEM], BF16, "mnT")
    zrow = S.sb([128, D], F32, "zrow")
    xn0 = S.sb([128, D], BF16, "xn0")
    ss0 = S.sb([128, 1], F32, "ss0")
    rs0 = S.sb([128, 1], F32, "rs0")
    P0 = [S.ps([128, 512], F32, f"p0{i}") for i in range(2)]
    PT0 = S.ps([128, 1024], BF16, "pt0")
    S.op("pool", lambda e: e.memset(zrow[:], 0.0), writes=[zrow])
    S.dma("sp", tab, tab.ap[0:128 * NB, :].rearrange("(p a) b -> p (a b)", p=128), zrow, zrow[:, 0:2 * NB].bitcast(I32))
    S.dma("sp", ybuf, ybuf.ap[NSLOT:NSLOT + 128, :], zrow, zrow[:])
    S.dma("pool", W_kv, W_kv[:], None, w_kv_d.rearrange("(c p) n -> p c n", p=128))
    S.dma("sp", mt, mt[:], None, mem_d.rearrange("(s p) d -> p s d", p=128))
    for s in range(2):
        S.op("act", lambda e, s=s: e.activation(out=xn0[:], in_=mt[:, s, :], func=AF.Square, accum_out=ss0[:, 0:1]),
             reads=[mt], writes=[xn0, ss0])
        rstd_from_ss(ss0, rs0, D)
        S.op("act", lambda e, s=s: e.activation(out=xn0[:], in_=mt[:, s, :], func=AF.Copy, scale=rs0[:, 0:1]),
             reads=[mt, rs0], writes=[xn0])
        for k in range(8):
            S.op("pe", lambda e, k=k: e.transpose(out=PT0[:, k * 128:(k + 1) * 128], in_=xn0[:, k * 128:(k + 1) * 128],
                                                  identity=identb[:]), reads=[xn0, identb], writes=[PT0])
        S.op("dve", lambda e, s=s: e.tensor_tensor(out=mnT[:, :, s * 128:(s + 1) * 128],
                                                   in0=PT0[:].rearrange("p (k t) -> p k t", k=8),
                                                   in1=P("gmT").unsqueeze(2).to_broadcast([128, 8, 128]), op=ALU.mult),
             reads=[PT0, PRM], writes=[mnT])
    pi = 0
    for c in range(8):
        ps = P0[pi % 2]
        pi += 1
        for k in range(8):
            S.op("pe", lambda e, c=c, k=k, ps=ps: e.matmul(ps[:, 0:MEM], lhsT=W_kv[:, k, c * 128:(c + 1) * 128], rhs=mnT[:, k, :],
                                                           start=(k == 0), stop=(k == 7)), reads=[W_kv, mnT], writes=[ps])
        S.op("act", lambda e, c=c, ps=ps: e.copy(out=KT[:, c, :], in_=ps[:, 0:MEM]), reads=[ps], writes=[KT])
    for mc in range(2):
        for hf in range(2):
            ps = P0[pi % 2]
            pi += 1
            for k in range(8):
                S.op("pe", lambda e, mc=mc, hf=hf, k=k, ps=ps: e.matmul(
                    ps[:, :], lhsT=mnT[:, k, mc * 128:(mc + 1) * 128], rhs=W_kv[:, k, D + hf * 512:D + (hf + 1) * 512],
                    start=(k == 0), stop=(k == 7)), reads=[W_kv, mnT], writes=[ps])
            S.op("dve", lambda e, mc=mc, hf=hf, ps=ps: e.tensor_copy(out=V[:, mc, hf * 512:(hf + 1) * 512], in_=ps[:, :]),
                 reads=[ps], writes=[V])
    S.flush()
    S.pop()
    S.dma("pool", W_out, W_out[:], None, w_out_d.rearrange("(c p) n -> p c n", p=128))
    S.dma("pool", W_q, W_q[:], None, w_q_d.rearrange("(c p) n -> p c n", p=128))
    S.dma("pool", W_o, W_o[:], None, w_o_d.rearrange("(c p) n -> p c n", p=128))

    PSA = S.ps([128, 512], F32, "psa")
    PSB = S.ps([128, 512], F32, "psb")
    PST = S.ps([128, 1024], BF16, "pst")
    PSW = S.ps([128, 1024], F32, "psw")
    PSS = S.ps([128, 512], F32, "pss")
    PSY = S.ps([128, 4, 128], F32, "psy")
    PSN = S.ps([128, 512], F32, "psn")
    PSR = PSS
    RO = 384
    rot = [PSA, PSB]
    rot_i = [0]

    def nextps():
        rot_i[0] += 1
        return rot[rot_i[0] % 2]

    XT = [S.sb([128, NS, D], F32, f"xt{i}") for i in range(2)]
    xn = S.sb([128, D], BF16, "xn")
    junk = xn
    ss = S.sb([128, 8], F32, "ss")
    rs = S.sb([128, 8], F32, "rs")
    GA = S.sb([128, 8, TS], BF16, "ga")
    GB = S.sb([128, 8, TS], BF16, "gb")
    GC = S.sb([128, 8, TS], BF16, "gc")
    xbc = S.sb([128, 6, TS + 3], F32, "xbc")
    gcv = S.sb([128, 4, TS + 2], F32, "gcv")
    gbf = S.sb([128, 4, TS], F32, "gbf")
    zs = S.sb([128, 4, TS], BF16, "zs")
    F4a = S.sb([128, 4 * TS], F32, "f4a")
    xsT = T(F4a.ap.rearrange("p (j t) -> p j t", j=4), "xsT", base=F4a)
    yT = xsT
    xsTb = S.sb([128, 4, TS], BF16, "xsTb")
    BCT = S.sb([128, 2, TS], BF16, "bct")
    CTz = S.sb([128, 2, TS], BF16, "ctz")
    tmpA = S.sb([128, TS], F32, "tmpA")
    tmpB = S.sb([128, TS], F32, "tmpB")
    tmpC = S.sb([128, TS], F32, "tmpC")
    dts = S.sb([128, NS, 8], F32, "dts")
    adt = S.sb([128, NS, 8], F32, "adt")
    adh = S.sb([128, NS, 8], BF16, "adh")
    adl = S.sb([128, NS, 8], BF16, "adl")
    sp1 = S.sb([128, NS * 8], F32, "sp1")
    sp2 = S.sb([128, NS * 8], F32, "sp2")
    css = S.sb([128, 48], F32, "css")
    F4b = S.sb([128, D], F32, "f4b")
    dif = T(F4b.ap.rearrange("p (h l) -> p h l", h=8), "dif", base=F4b)
    Dm = S.sb([128, 8, 128], BF16, "Dm")
    Mm = S.sb([128, 8, 128], BF16, "Mm")
    cbm = S.sb([128, 2, 128], F32, "cbm")
    Ecs = S.sb([128, 4, 128], BF16, "Ecs")
    CTs = S.sb([128, 4, 128], BF16, "CTs")
    xdt = S.sb([128, 512], BF16, "xdt")
    xdts = S.sb([128, 512], BF16, "xdts")
    Btok = S.sb([128, 128], BF16, "Btok")
    Sst = S.sb([128, 512], F32, "Sst")
    Sbf = [S.sb([128, 512], BF16, f"Sbf{i}") for i in range(2)]
    GA2 = S.sb([128, 8, TS], BF16, "ga2")
    GB2f = S.sb([128, 4 * TS], F32, "gb2f")
    GB2 = T(GB2f.ap.bitcast(BF16).rearrange("p (c t) -> p c t", c=8), "gb2", base=GB2f)
    xn2 = S.sb([128, D], BF16, "xn2")
    ss2 = S.sb([128, 8], F32, "ss2")
    rs2 = S.sb([128, 8], F32, "rs2")
    mx = S.sb([128, 8], F32, "mx")
    Pf = S.sb([128, 4, MEM], BF16, "Pf")
    Pn = S.sb([128, 4, MEM], BF16, "Pn")
    n3 = T(GB2f.ap[:, 0:D], "n3", base=GB2f)
    n3b = S.sb([128, D], BF16, "n3b")
    L = S.sb([128, 36], F32, "L")
    rt = S.sb([128, 160], F32, "rt")
    A0 = S.sb([128, 32], F32, "A0")
    A1 = S.sb([128, 32], F32, "A1")
    Ab = S.sb([128, 32], BF16, "Ab")
    cnt = S.sb([128, 32], F32, "cnt")
    posf = S.sb([128, 32], F32, "posf")
    pki = S.sb([128, 8], I32, "pki")
    ent = [S.sb([128, 2], I32, f"ent{i}") for i in range(4)]
    sidx = [S.sb([128, 1], I32, f"sidx{i}") for i in range(4)]

    if _os.environ.get("K_TRACE"):
        print("SBUF remaining at pass-A peak:", nc.sbuf_bytes_remaining)
    S.op("pool", lambda e: e.memset(xbc[:, :, 0:3], 0.0), writes=[xbc])
    S.op("pool", lambda e: e.memset(gcv[:, :, 0:2], 0.0), writes=[gcv])
    S.op("pool", lambda e: e.memset(Sst[:], 0.0), writes=[Sst])
    S.op("pool", lambda e: e.memset(Sbf[0][:], 0.0), writes=[Sbf[0]])
    S.op("pool", lambda e: e.memset(Sbf[1][:], 0.0), writes=[Sbf[1]])
    S.op("pool", lambda e: e.memset(CTz[:], 0.0), writes=[CTz])
    S.op("pool", lambda e: e.memset(cnt[:], 0.0), writes=[cnt])
    sbf_i = [0]

    def load_x(st):
        xt = XT[st % 2]
        S.dma("sp", xt, xt[:], None, x_d[st * TS:(st + 1) * TS, :].rearrange("(s p) d -> p s d", p=128))

    def norm_T(src_t, gname, dst, xn=xn, ss=ss, rs=rs):
        for s in range(NS):
            S.op("act", lambda e, s=s: e.activation(out=xn[:], in_=src_t[:, s, :], func=AF.Square, accum_out=ss[:, s:s + 1]),
                 reads=[src_t], writes=[xn, ss])
        rstd_from_ss(ss, rs, D, NS)
        for s in range(NS):
            S.op("act", lambda e, s=s: e.activation(out=xn[:], in_=src_t[:, s, :], func=AF.Copy, scale=rs[:, s:s + 1]),
                 reads=[src_t, rs], writes=[xn])
            for k in range(8):
                S.op("pe", lambda e, k=k: e.transpose(out=PST[:, k * 128:(k + 1) * 128], in_=xn[:, k * 128:(k + 1) * 128],
                                                      identity=identb[:]), reads=[xn, identb], writes=[PST])
            S.op("dve", lambda e, s=s: e.tensor_tensor(out=dst[:, :, s * 128:(s + 1) * 128],
                                                       in0=PST[:].rearrange("p (k t) -> p k t", k=8),
                                                       in1=P(gname).unsqueeze(2).to_broadcast([128, 8, 128]), op=ALU.mult),
                 reads=[PST, PRM], writes=[dst])

    def proj_B(Wt, c0, rhsT, ps, n=TS):
        for k in range(8):
            S.op("pe", lambda e, k=k: e.matmul(ps[:, 0:n], lhsT=Wt[:, k, c0:c0 + 128], rhs=rhsT[:, k, 0:n],
                                               start=(k == 0), stop=(k == 7)), reads=[Wt, rhsT], writes=[ps])

    def proj_A(Wt, lhsT_t, s, nk=8):
        for hf in range(2):
            for k in range(nk):
                S.op("pe", lambda e, k=k, hf=hf: e.matmul(PSW[:, hf * 512:(hf + 1) * 512], lhsT=lhsT_t[:, k, s * 128:(s + 1) * 128],
                                                          rhs=Wt[:, k, hf * 512:(hf + 1) * 512], start=(k == 0), stop=(k == nk - 1)),
                     reads=[Wt, lhsT_t], writes=[PSW])

    def gen_M(st):
        xt = XT[st % 2]
        if _os.environ.get("K_TRACE"): print("MARK", st, "norm1", S.nrec)
        norm_T(xt, "g1T", GA)
        if _os.environ.get("K_TRACE"): print("MARK", st, "inproj", S.nrec)
        for s in range(NS):
            yield
            for k in range(8):
                S.op("pe", lambda e, k=k, s=s: e.matmul(PSS[:, s * 8:(s + 1) * 8], lhsT=GA[:, k, s * 128:(s + 1) * 128],
                                                        rhs=W_in[:, k, 1280:1288], start=(k == 0), stop=(k == 7)),
                     reads=[GA, W_in], writes=[PSS])
        S.op("dve", lambda e: e.tensor_tensor(out=sp1[:].rearrange("p (s h) -> p s h", h=8),
                                              in0=PSS[:, 0:NS * 8].rearrange("p (s h) -> p s h", h=8),
                                              in1=P("dtb").unsqueeze(1).to_broadcast([128, NS, 8]), op=ALU.add),
             reads=[PSS, PRM], writes=[sp1])
        S.op("act", lambda e: e.activation(out=sp2[:], in_=sp1[:], func=AF.Abs), reads=[sp1], writes=[sp2])
        S.op("act", lambda e: e.activation(out=sp2[:], in_=sp2[:], func=AF.Exp, scale=-1.0), reads=[sp2], writes=[sp2])
        S.op("act", lambda e: e.activation(out=sp2[:], in_=sp2[:], func=AF.Ln, bias=1.0, scale=1.0), reads=[sp2], writes=[sp2])
        S.op("dve", lambda e: e.tensor_scalar(out=sp1[:], in0=sp1[:], scalar1=0.0, scalar2=None, op0=ALU.max),
             reads=[sp1], writes=[sp1])
        S.op("dve", lambda e: e.tensor_tensor(out=dts[:].rearrange("p s h -> p (s h)"), in0=sp1[:], in1=sp2[:], op=ALU.add),
             reads=[sp1, sp2], writes=[dts])
        S.op("dve", lambda e: e.tensor_tensor(out=adt[:], in0=dts[:], in1=DRV[:, 32:40].unsqueeze(1).to_broadcast([128, NS, 8]),
                                              op=ALU.mult), reads=[dts, DRV], writes=[adt])
        S.op("dve", lambda e: e.tensor_copy(out=adh[:], in_=adt[:]), reads=[adt], writes=[adh])
        S.op("dve", lambda e: e.tensor_tensor(out=adl[:], in0=adt[:], in1=adh[:], op=ALU.subtract), reads=[adt, adh], writes=[adl])
        if _os.environ.get("K_TRACE"): print("MARK", st, "z", S.nrec)
        for j in range(4):
            yield
            ps = nextps()
            proj_B(W_in, j * 128, GA, ps)
            S.op("act", lambda e, ps=ps: e.activation(out=tmpA[:], in_=ps[:, 0:TS], func=AF.Tanh, scale=0.5), reads=[ps], writes=[tmpA])
            S.op("act", lambda e, ps=ps: e.activation(out=tmpB[:], in_=ps[:, 0:TS], func=AF.Copy, scale=0.5), reads=[ps], writes=[tmpB])
            S.op("dve", lambda e, j=j: e.scalar_tensor_tensor(out=zs[:, j, :], in0=tmpA[:], scalar=1.0, in1=tmpB[:],
                                                              op0=ALU.add, op1=ALU.mult), reads=[tmpA, tmpB], writes=[zs])
        for j in range(6):
            yield
            ps = nextps()
            proj_B(W_in, 512 + j * 128, GA, ps)
            S.op("act", lambda e, ps=ps, j=j: e.copy(out=xbc[:, j, 3:3 + TS], in_=ps[:, 0:TS]), reads=[ps], writes=[xbc])
        for j in range(4):
            yield
            ps = nextps()
            proj_B(W_in, 1288 + j * 128, GA, ps)
            S.op("act", lambda e, ps=ps, j=j: e.copy(out=gbf[:, j, :], in_=ps[:, 0:TS]), reads=[ps], writes=[gbf])
        for j in range(4):
            yield
            ps = nextps()
            proj_B(W_in, 1800 + j * 128, GA, ps)
            S.op("act", lambda e, ps=ps: e.copy(out=tmpA[:], in_=ps[:, 0:TS]), reads=[ps], writes=[tmpA])
            ps2 = nextps()
            proj_B(W_in, 2312 + j * 128, GA, ps2)
            S.op("dve", lambda e, ps2=ps2, j=j: e.tensor_tensor(out=gcv[:, j, 2:2 + TS], in0=tmpA[:], in1=ps2[:, 0:TS], op=ALU.mult),
                 reads=[tmpA, ps2], writes=[gcv])
        if _os.environ.get("K_TRACE"): print("MARK", st, "conv4", S.nrec)
        for j in range(6):
            yield
            S.op("dve", lambda e, j=j: e.tensor_scalar(out=tmpA[:], in0=xbc[:, j, 0:TS], scalar1=DRV[:, j * 4:j * 4 + 1],
                                                       scalar2=DRV[:, 24 + j:25 + j], op0=ALU.mult, op1=ALU.add),
                 reads=[xbc, DRV], writes=[tmpA])
            for i in range(1, 4):
                S.op("dve", lambda e, j=j, i=i: e.scalar_tensor_tensor(out=tmpA[:], in0=xbc[:, j, i:i + TS],
                                                                       scalar=DRV[:, j * 4 + i:j * 4 + i + 1], in1=tmpA[:],
                                                                       op0=ALU.mult, op1=ALU.add), reads=[xbc, DRV, tmpA], writes=[tmpA])
            S.op("act", lambda e: e.activation(out=tmpB[:], in_=tmpA[:], func=AF.Tanh), reads=[tmpA], writes=[tmpB])
            if j < 4:
                S.op("dve", lambda e, j=j: e.scalar_tensor_tensor(out=xsT[:, j, :], in0=tmpB[:], scalar=1.0, in1=tmpA[:],
                                                                  op0=ALU.add, op1=ALU.mult), reads=[tmpA, tmpB], writes=[xsT])
                S.op("act", lambda e, j=j: e.copy(out=xsTb[:, j, :], in_=xsT[:, j, :]), reads=[xsT], writes=[xsTb])
            else:
                S.op("dve", lambda e, j=j: e.scalar_tensor_tensor(out=BCT[:, j - 4, :], in0=tmpB[:], scalar=1.0, in1=tmpA[:],
                                                                  op0=ALU.add, op1=ALU.mult), reads=[tmpA, tmpB], writes=[BCT])
        for g in range(2):
            yield
            S.op("act", lambda e, g=g: e.copy(out=CTz[g * 64:(g + 1) * 64, g, :], in_=BCT[g * 64:(g + 1) * 64, 1, :]), reads=[BCT], writes=[CTz])
        S.op("dve", lambda e: e.tensor_copy(out=xbc[:, :, 0:3], in_=xbc[:, :, TS:TS + 3]), reads=[xbc], writes=[xbc])
        if _os.environ.get("K_TRACE"): print("MARK", st, "sconv", S.nrec)
        for j in range(4):
            yield
            o = offs["sw"][0] + j * 3
            S.op("dve", lambda e, j=j, o=o: e.tensor_scalar(out=tmpC[:], in0=gcv[:, j, 0:TS], scalar1=PRM[:, o:o + 1], scalar2=None,
                                                            op0=ALU.mult), reads=[gcv, PRM], writes=[tmpC])
            for i in range(1, 3):
                S.op("dve", lambda e, j=j, i=i, o=o: e.scalar_tensor_tensor(out=tmpC[:], in0=gcv[:, j, i:i + TS],
                                                                            scalar=PRM[:, o + i:o + i + 1], in1=tmpC[:],
                                                                            op0=ALU.mult, op1=ALU.add), reads=[gcv, PRM, tmpC], writes=[tmpC])
            S.op("dve", lambda e, j=j: e.tensor_tensor(out=GB[:, 4 + j, :], in0=tmpC[:], in1=gbf[:, j, :], op=ALU.mult),
                 reads=[tmpC, gbf], writes=[GB])
        S.op("dve", lambda e: e.tensor_copy(out=gcv[:, :, 0:2], in_=gcv[:, :, TS:TS + 2]), reads=[gcv], writes=[gcv])
        if _os.environ.get("K_TRACE"): print("MARK", st, "ssd", S.nrec)
        for s in range(NS):
            yield
            sl = slice(s * 128, (s + 1) * 128)
            sprev = Sbf[sbf_i[0] % 2]
            snext = Sbf[(sbf_i[0] + 1) % 2]
            sbf_i[0] += 1
            for ii, ad in enumerate((adh, adl)):
                S.op("pe", lambda e, s=s, ad=ad, ii=ii: e.matmul(PSS[:, 32:40], lhsT=trib[:], rhs=ad[:, s, :], start=(ii == 0), stop=(ii == 1)),
                     reads=[trib, ad], writes=[PSS])
            for ii, ad in enumerate((adh, adl)):
                S.op("pe", lambda e, s=s, ad=ad, ii=ii: e.matmul(PSS[:, 40:48], lhsT=onesb[:], rhs=ad[:, s, :], start=(ii == 0), stop=(ii == 1)),
                     reads=[onesb, ad], writes=[PSS])
            for h in range(8):
                for ii, ad in enumerate((adh, adl)):
                    S.op("pe", lambda e, s=s, h=h, ad=ad, ii=ii: e.matmul(PSW[:, h * 128:(h + 1) * 128], lhsT=ad[:, s, h:h + 1].to_broadcast([128, 128]),
                                                                          rhs=trib[:], start=(ii == 0), stop=(ii == 1)), reads=[trib, ad], writes=[PSW])
            yield
            for j in range(4):
                S.op("pe", lambda e, j=j, sl=sl: e.transpose(out=PST[:, j * 128:(j + 1) * 128], in_=xsTb[:, j, sl], identity=identb[:]),
                     reads=[xsTb, identb], writes=[PST])
            S.op("pe", lambda e, sl=sl: e.transpose(out=PST[:, 512:640], in_=BCT[:, 0, sl], identity=identb[:]),
                 reads=[BCT, identb], writes=[PST])
            for g in range(2):
                S.op("pe", lambda e, g=g, sl=sl: e.matmul(PSS[:, 128 + g * 128:256 + g * 128], lhsT=BCT[:, 0, sl],
                                                          rhs=CTz[:, g, sl], start=True, stop=True),
                     reads=[BCT, CTz], writes=[PSS])
            S.op("dve", lambda e: e.tensor_copy(out=css[:, 0:8], in_=PSS[:, 32:40]), reads=[PSS], writes=[css])
            S.op("dve", lambda e: e.tensor_tensor(out=css[:, 8:16], in0=PSS[:, 40:48], in1=css[:, 0:8], op=ALU.subtract),
                 reads=[PSS, css], writes=[css])
            S.op("act", lambda e: e.activation(out=css[:, 8:16], in_=css[:, 8:16], func=AF.Exp), reads=[css], writes=[css])
            S.op("act", lambda e: e.activation(out=css[:, 24:32], in_=PSS[:, 40:48], func=AF.Exp), reads=[PSS], writes=[css])
            S.op("dve", lambda e, s=s: e.tensor_tensor(out=css[:, 16:24], in0=css[:, 8:16], in1=dts[:, s, :], op=ALU.mult),
                 reads=[css, dts], writes=[css])
            yield
            for h in range(8):
                S.op("dve", lambda e, h=h: e.tensor_scalar(out=dif[:, h, :], in0=PSW[:, h * 128:(h + 1) * 128], scalar1=css[:, h:h + 1],
                                                           scalar2=0.0, op0=ALU.subtract, op1=ALU.min), reads=[PSW, css], writes=[dif])
            S.op("act", lambda e: e.activation(out=Dm[:], in_=dif[:], func=AF.Exp), reads=[dif], writes=[Dm])
            S.op("dve", lambda e: e.tensor_tensor(out=cbm[:], in0=PSS[:, 128:384].rearrange("p (g l) -> p g l", g=2),
                                                  in1=P("tri").unsqueeze(1).to_broadcast([128, 2, 128]), op=ALU.mult),
                 reads=[PSS, PRM], writes=[cbm])
            S.op("dve", lambda e: e.tensor_tensor(out=Mm[:].rearrange("p (g r) l -> p g r l", g=2),
                                                  in0=Dm[:].rearrange("p (g r) l -> p g r l", g=2),
                                                  in1=cbm[:].unsqueeze(2).to_broadcast([128, 2, 4, 128]), op=ALU.mult),
                 reads=[Dm, cbm], writes=[Mm])
            for g in range(2):
                S.op("act", lambda e, g=g: e.activation(out=Ecs[g * 64:(g + 1) * 64, :, :],
                                                        in_=PSW[g * 64:(g + 1) * 64, g * 512:(g + 1) * 512].rearrange("p (r l) -> p r l", r=4),
                                                        func=AF.Exp), reads=[PSW], writes=[Ecs])
                S.op("dve", lambda e, g=g, sl=sl: e.tensor_tensor(out=CTs[g * 64:(g + 1) * 64, :, :], in0=Ecs[g * 64:(g + 1) * 64, :, :],
                                                                  in1=BCT[g * 64:(g + 1) * 64, 1, sl].unsqueeze(1).to_broadcast([64, 4, 128]),
                                                                  op=ALU.mult), reads=[Ecs, BCT], writes=[CTs])
            yield
            S.op("dve", lambda e, s=s: e.tensor_tensor(out=xdt[:].rearrange("p (h q) -> p h q", h=8),
                                                       in0=PST[:, 0:512].rearrange("p (h q) -> p h q", h=8),
                                                       in1=dts[:, s, :].unsqueeze(2).to_broadcast([128, 8, 64]), op=ALU.mult),
                 reads=[PST, dts], writes=[xdt])
            S.op("dve", lambda e: e.tensor_tensor(out=xdts[:].rearrange("p (h q) -> p h q", h=8),
                                                  in0=PST[:, 0:512].rearrange("p (h q) -> p h q", h=8),
                                                  in1=css[:, 16:24].unsqueeze(2).to_broadcast([128, 8, 64]), op=ALU.mult),
                 reads=[PST, css], writes=[xdts])
            S.op("act", lambda e: e.copy(out=Btok[:], in_=PST[:, 512:640]), reads=[PST], writes=[Btok])
            yield
            for h in range(8):
                g = h // 4
                r = h % 4
                osl = slice((h % 2) * 64, (h % 2) * 64 + 64)
                S.op("pe", lambda e, h=h, osl=osl: e.matmul(PSY[osl, h // 2, :], lhsT=xdt[:, h * 64:(h + 1) * 64], rhs=Mm[:, h, :],
                                                            start=True, stop=False), reads=[xdt, Mm], writes=[PSY])
                S.op("pe", lambda e, h=h, g=g, r=r, osl=osl: e.matmul(PSY[osl, h // 2, :], lhsT=sprev[:, h * 64:(h + 1) * 64],
                                                                      rhs=CTs[:, r, :], start=False, stop=True),
                     reads=[sprev, CTs], writes=[PSY])
            for j in range(4):
                o = offs["dsk"][0] + j
                S.op("dve", lambda e, j=j, o=o, sl=sl: e.scalar_tensor_tensor(out=yT[:, j, sl], in0=xsT[:, j, sl], scalar=PRM[:, o:o + 1],
                                                                              in1=PSY[:, j, :], op0=ALU.mult, op1=ALU.add),
                     reads=[xsT, PRM, PSY], writes=[yT])
            yield
            S.op("pe", lambda e: e.matmul(PSN[:, :], lhsT=Btok[:], rhs=xdts[:], start=True, stop=True), reads=[Btok, xdts], writes=[PSN])
            S.op("dve", lambda e: e.tensor_tensor(out=Sst[:].rearrange("p (h q) -> p h q", h=8), in0=Sst[:].rearrange("p (h q) -> p h q", h=8),
                                                  in1=css[:, 24:32].unsqueeze(2).to_broadcast([128, 8, 64]), op=ALU.mult),
                 reads=[Sst, css], writes=[Sst])
            S.op("dve", lambda e: e.tensor_tensor(out=Sst[:], in0=Sst[:], in1=PSN[:, :], op=ALU.add), reads=[Sst, PSN], writes=[Sst])
            for g in range(2):
                S.op("act", lambda e, snext=snext, g=g: e.copy(out=snext[g * 64:(g + 1) * 64, g * 256:(g + 1) * 256],
                                                               in_=Sst[g * 64:(g + 1) * 64, g * 256:(g + 1) * 256]), reads=[Sst], writes=[snext])
        if _os.environ.get("K_TRACE"): print("MARK", st, "gate", S.nrec)
        for j in range(4):
            yield
            S.op("dve", lambda e, j=j: e.tensor_tensor(out=yT[:, j, :], in0=yT[:, j, :], in1=zs[:, j, :], op=ALU.mult),
                 reads=[yT, zs], writes=[yT])
            S.op("act", lambda e, j=j: e.activation(out=xsTb[:, j, :], in_=yT[:, j, :], func=AF.Square), reads=[yT], writes=[xsTb])
        for g in range(2):
            yield
            ps = nextps()
            for jj in range(2):
                S.op("pe", lambda e, g=g, jj=jj, ps=ps: e.matmul(ps[:, 0:TS], lhsT=onesb[:], rhs=xsTb[:, 2 * g + jj, :],
                                                                 start=(jj == 0), stop=(jj == 1)), reads=[onesb, xsTb], writes=[ps])
            S.op("dve", lambda e, ps=ps: e.tensor_scalar(out=tmpA[:], in0=ps[:, 0:TS], scalar1=1.0 / 256, scalar2=EPS,
                                                         op0=ALU.mult, op1=ALU.add), reads=[ps], writes=[tmpA])
            S.op("act", lambda e: e.activation(out=tmpA[:], in_=tmpA[:], func=AF.Ln), reads=[tmpA], writes=[tmpA])
            S.op("act", lambda e: e.activation(out=tmpA[:], in_=tmpA[:], func=AF.Exp, scale=-0.5), reads=[tmpA], writes=[tmpA])
            for jj in range(2):
                j = 2 * g + jj
                o = offs["ggT"][0] + j
                S.op("dve", lambda e, j=j, o=o: e.scalar_tensor_tensor(out=GB[:, j, :], in0=yT[:, j, :], scalar=PRM[:, o:o + 1],
                                                                       in1=tmpA[:], op0=ALU.mult, op1=ALU.mult),
                     reads=[yT, PRM, tmpA], writes=[GB])
        if _os.environ.get("K_TRACE"): print("MARK", st, "outproj", S.nrec)
        for s in range(NS):
            yield
            proj_A(W_out, GB, s)
            S.op("dve", lambda e, s=s: e.tensor_tensor(out=xt[:, s, :], in0=xt[:, s, :], in1=PSW[:, :], op=ALU.add),
                 reads=[xt, PSW], writes=[xt])
            if dbg:
                S.dma("sp", None, dbg_h1[st * TS + s * 128:st * TS + (s + 1) * 128, :], xt, xt[:, s, :])

    def gen_X(st):
        xt = XT[st % 2]
        if _os.environ.get("K_TRACE"): print("MARK", st, "attn", S.nrec)
        norm_T(xt, "g2T", GA2, xn=xn2, ss=ss2, rs=rs2)
        for c in range(8):
            yield
            ps = nextps()
            proj_B(W_q, c * 128, GA2, ps)
            S.op("act", lambda e, c=c, ps=ps: e.activation(out=GC[:, c, :], in_=ps[:, 0:TS], func=AF.Copy, scale=1.0 / 16),
                 reads=[ps], writes=[GC])
        for s in range(NS):
            yield
            sl = slice(s * 128, (s + 1) * 128)
            for h in range(4):
                for kk in range(2):
                    S.op("pe", lambda e, h=h, kk=kk, sl=sl: e.matmul(PSW[:, h * MEM:(h + 1) * MEM], lhsT=GC[:, 2 * h + kk, sl],
                                                                     rhs=KT[:, 2 * h + kk, :], start=(kk == 0), stop=(kk == 1)),
                         reads=[GC, KT], writes=[PSW])
            S.op("dve", lambda e: e.tensor_reduce(out=mx[:, 0:4], in_=PSW[:, :].rearrange("p (h m) -> p h m", h=4), axis=AX.X, op=ALU.max),
                 reads=[PSW], writes=[mx])
            S.op("dve", lambda e: e.tensor_scalar(out=mx[:, 0:4], in0=mx[:, 0:4], scalar1=-1.0, scalar2=None, op0=ALU.mult),
                 reads=[mx], writes=[mx])
            for h in range(4):
                S.op("act", lambda e, h=h: e.activation(out=Pf[:, h, :], in_=PSW[:, h * MEM:(h + 1) * MEM], func=AF.Exp,
                                                        bias=mx[:, h:h + 1], scale=1.0, accum_out=mx[:, 4 + h:5 + h]),
                     reads=[PSW, mx], writes=[Pf, mx])
            S.op("dve", lambda e: e.reciprocal(out=mx[:, 4:8], in_=mx[:, 4:8]), reads=[mx], writes=[mx])
            S.op("dve", lambda e: e.tensor_tensor(out=Pn[:], in0=Pf[:], in1=mx[:, 4:8].unsqueeze(2).to_broadcast([128, 4, MEM]), op=ALU.mult),
                 reads=[Pf, mx], writes=[Pn])
            for c in range(8):
                S.op("pe", lambda e, c=c: e.transpose(out=PST[:, c * 128:(c + 1) * 128], in_=Pn[:, c // 2, (c % 2) * 128:(c % 2 + 1) * 128],
                                                      identity=identb[:]), reads=[Pn, identb], writes=[PST])
            S.op("act", lambda e, sl=sl: e.copy(out=GB2[:, :, sl], in_=PST[:].rearrange("p (c t) -> p c t", c=8)), reads=[PST], writes=[GB2])
        for c in range(8):
            yield
            ps = nextps()
            h = c // 2
            for mc in range(2):
                S.op("pe", lambda e, c=c, h=h, mc=mc, ps=ps: e.matmul(ps[:, 0:TS], lhsT=V[:, mc, c * 128:(c + 1) * 128], rhs=GB2[:, 2 * h + mc, :],
                                                                      start=(mc == 0), stop=(mc == 1)), reads=[V, GB2], writes=[ps])
            S.op("act", lambda e, c=c, ps=ps: e.copy(out=GA2[:, c, :], in_=ps[:, 0:TS]), reads=[ps], writes=[GA2])
        for s in range(NS):
            yield
            proj_A(W_o, GA2, s)
            S.op("dve", lambda e, s=s: e.tensor_tensor(out=xt[:, s, :], in0=xt[:, s, :], in1=PSW[:, :], op=ALU.add),
                 reads=[xt, PSW], writes=[xt])
            tok0 = st * TS + s * 128
            S.dma("sp", h2buf, h2buf.ap[tok0:tok0 + 128, :], xt, xt[:, s, :])
            if dbg:
                S.dma("sp", None, dbg_h2[tok0:tok0 + 128, :], xt, xt[:, s, :])
        if _os.environ.get("K_TRACE"): print("MARK", st, "route", S.nrec)
        for s in range(NS if stop != "A0" else 0):
            yield
            S.op("act", lambda e, s=s: e.activation(out=xn2[:], in_=xt[:, s, :], func=AF.Square, accum_out=ss2[:, s:s + 1]),
                 reads=[xt], writes=[xn2, ss2])
        rstd_from_ss(ss2, rs2, D, NS)
        for s in range(NS if stop != "A0" else 0):
            yield
            tile_i = st * NS + s
            tok0 = tile_i * 128
            S.op("dve", lambda e, s=s: e.scalar_tensor_tensor(out=n3[:], in0=xt[:, s, :], scalar=rs2[:, s:s + 1], in1=G3[:],
                                                              op0=ALU.mult, op1=ALU.mult), reads=[xt, rs2, G3], writes=[n3])
            S.op("act", lambda e: e.copy(out=n3b[:], in_=n3[:]), reads=[n3], writes=[n3b])
            S.dma("sp", n3buf, n3buf.ap[tok0:tok0 + 128, :], n3b, n3b[:])
            yield
            S.op("dve", lambda e: e.tensor_tensor(out=xn2[:], in0=n3[:], in1=n3b[:], op=ALU.subtract), reads=[n3, n3b], writes=[xn2])
            for src, dstT in ((n3b, GA2), (xn2, GC)):
                for k in range(8):
                    S.op("pe", lambda e, k=k, src=src: e.transpose(out=PST[:, k * 128:(k + 1) * 128], in_=src[:, k * 128:(k + 1) * 128], identity=identb[:]),
                         reads=[src, identb], writes=[PST])
                S.op("act", lambda e, dstT=dstT: e.copy(out=dstT[:, :, 0:128], in_=PST[:].rearrange("p (k t) -> p k t", k=8)), reads=[PST], writes=[dstT])
            combos = [(GA2, W_rh), (GC, W_rh), (GA2, W_rl)]
            for ci, (aT, wr) in enumerate(combos):
                for k in range(8):
                    S.op("pe", lambda e, k=k, aT=aT, wr=wr, ci=ci: e.matmul(PSS[:, RO:RO + 36], lhsT=aT[:, k, 0:128], rhs=wr[:, k, :],
                                                                            start=(ci == 0 and k == 0), stop=(ci == 2 and k == 7)),
                         reads=[aT, wr], writes=[PSS])
            S.op("dve", lambda e: e.tensor_tensor(out=L[:], in0=PSS[:, RO:RO + 36], in1=P("br"), op=ALU.add), reads=[PSS, PRM], writes=[L])
            S.op("dve", lambda e: e.tensor_reduce(out=rt[:, 0:1], in_=L[:, 0:4], axis=AX.X, op=ALU.max), reads=[L], writes=[rt])
            S.op("dve", lambda e: e.tensor_scalar(out=rt[:, 1:2], in0=rt[:, 0:1], scalar1=-1.0, scalar2=None, op0=ALU.mult), reads=[rt], writes=[rt])
            S.op("dve", lambda e: e.tensor_scalar(out=rt[:, 4:8], in0=L[:, 0:4], scalar1=rt[:, 0:1], scalar2=None, op0=ALU.is_equal),
                 reads=[L, rt], writes=[rt])
            S.op("act", lambda e: e.activation(out=rt[:, 32:36], in_=L[:, 0:4], func=AF.Exp, bias=rt[:, 1:2], scale=1.0, accum_out=rt[:, 2:3]),
                 reads=[L, rt], writes=[rt])
            S.op("dve", lambda e: e.reciprocal(out=rt[:, 3:4], in_=rt[:, 2:3]), reads=[rt], writes=[rt])
            S.op("dve", lambda e: e.tensor_scalar(out=rt[:, 8:12], in0=rt[:, 4:8], scalar1=-1.0, scalar2=1e9, op0=ALU.add, op1=ALU.mult),
                 reads=[rt], writes=[rt])
            S.op("dve", lambda e: e.tensor_tensor(out=rt[:, 64:96].rearrange("p (g j) -> p g j", g=4), in0=L[:, 4:36].rearrange("p (g j) -> p g j", g=4),
                                                  in1=rt[:, 8:12].unsqueeze(2).to_broadcast([128, 4, 8]), op=ALU.add), reads=[L, rt], writes=[rt])
            S.op("dve", lambda e: e.max(out=rt[:, 12:20], in_=rt[:, 64:96]), reads=[rt], writes=[rt])
            S.op("dve", lambda e: e.tensor_scalar(out=A0[:], in0=rt[:, 64:96], scalar1=rt[:, 12:13], scalar2=None, op0=ALU.is_equal),
                 reads=[rt], writes=[A0])
            S.op("dve", lambda e: e.tensor_scalar(out=A1[:], in0=rt[:, 64:96], scalar1=rt[:, 13:14], scalar2=None, op0=ALU.is_equal),
                 reads=[rt], writes=[A1])
            S.op("dve", lambda e: e.tensor_tensor(out=Ab[:], in0=A0[:], in1=A1[:], op=ALU.add), reads=[A0, A1], writes=[Ab])
            yield
            S.op("dve", lambda e: e.tensor_tensor(out=rt[:, 20:21], in0=rt[:, 13:14], in1=rt[:, 12:13], op=ALU.subtract), reads=[rt], writes=[rt])
            S.op("act", lambda e: e.activation(out=rt[:, 21:22], in_=rt[:, 20:21], func=AF.Exp), reads=[rt], writes=[rt])
            S.op("dve", lambda e: e.tensor_scalar(out=rt[:, 22:23], in0=rt[:, 21:22], scalar1=1.0, scalar2=None, op0=ALU.add), reads=[rt], writes=[rt])
            S.op("dve", lambda e: e.reciprocal(out=rt[:, 22:23], in_=rt[:, 22:23]), reads=[rt], writes=[rt])
            S.op("dve", lambda e: e.tensor_tensor(out=rt[:, 23:24], in0=rt[:, 21:22], in1=rt[:, 22:23], op=ALU.mult), reads=[rt], writes=[rt])
            S.op("dve", lambda e: e.tensor_scalar(out=rt[:, 24:26], in0=rt[:, 22:24], scalar1=rt[:, 3:4], scalar2=None, op0=ALU.mult),
                 reads=[rt], writes=[rt])
            yield
            S.op("pe", lambda e: e.matmul(PSS[:, RO + 36:RO + 68], lhsT=trisb[:], rhs=Ab[:], start=True, stop=True), reads=[trisb, Ab], writes=[PSS])
            S.op("pe", lambda e: e.matmul(PSS[:, RO + 68:RO + 100], lhsT=onesb[:], rhs=Ab[:], start=True, stop=True), reads=[onesb, Ab], writes=[PSS])
            S.op("dve", lambda e: e.tensor_tensor(out=posf[:], in0=PSS[:, RO + 36:RO + 68], in1=cnt[:], op=ALU.add), reads=[PSS, cnt], writes=[posf])
            S.op("dve", lambda e: e.tensor_tensor(out=cnt[:], in0=cnt[:], in1=PSS[:, RO + 68:RO + 100], op=ALU.add), reads=[PSS, cnt], writes=[cnt])
            for k, Ak in enumerate((A0, A1)):
                S.op("dve", lambda e, Ak=Ak: e.tensor_tensor(out=rt[:, 96:128], in0=Ak[:], in1=posf[:], op=ALU.mult), reads=[Ak, posf], writes=[rt])
                S.op("dve", lambda e, k=k: e.tensor_reduce(out=rt[:, 26 + k:27 + k], in_=rt[:, 96:128], axis=AX.X, op=ALU.add), reads=[rt], writes=[rt])
                S.op("dve", lambda e, Ak=Ak: e.tensor_tensor(out=rt[:, 96:128], in0=Ak[:], in1=P("eidx"), op=ALU.mult), reads=[Ak, PRM], writes=[rt])
                S.op("dve", lambda e, k=k: e.tensor_reduce(out=rt[:, 28 + k:29 + k], in_=rt[:, 96:128], axis=AX.X, op=ALU.add), reads=[rt], writes=[rt])
            S.op("dve", lambda e: e.tensor_scalar(out=rt[:, 30:32], in0=rt[:, 26:28], scalar1=float(CAP), scalar2=None, op0=ALU.is_lt), reads=[rt], writes=[rt])
            S.op("dve", lambda e: e.tensor_scalar(out=rt[:, 36:38], in0=rt[:, 30:32], scalar1=-1.0, scalar2=-1.0, op0=ALU.add, op1=ALU.mult),
                 reads=[rt], writes=[rt])
            yield
            S.op("dve", lambda e: e.tensor_copy(out=pki[:, 0:2], in_=rt[:, 26:28]), reads=[rt], writes=[pki])
            S.op("dve", lambda e: e.tensor_scalar(out=pki[:, 2:4], in0=pki[:, 0:2], scalar1=7, scalar2=None, op0=ALU.arith_shift_right),
                 reads=[pki], writes=[pki])
            S.op("dve", lambda e: e.tensor_scalar(out=pki[:, 4:6], in0=pki[:, 0:2], scalar1=127, scalar2=None, op0=ALU.bitwise_and),
                 reads=[pki], writes=[pki])
            S.op("dve", lambda e: e.tensor_copy(out=rt[:, 40:44], in_=pki[:, 2:6]), reads=[pki], writes=[rt])
            S.op("dve", lambda e: e.tensor_scalar(out=rt[:, 44:46], in0=rt[:, 42:44], scalar1=float(NB), scalar2=None, op0=ALU.mult), reads=[rt], writes=[rt])
            S.op("dve", lambda e: e.scalar_tensor_tensor(out=rt[:, 44:46], in0=rt[:, 28:30], scalar=float(CB), in1=rt[:, 44:46], op0=ALU.mult, op1=ALU.add),
                 reads=[rt], writes=[rt])
            S.op("dve", lambda e: e.tensor_tensor(out=rt[:, 44:46], in0=rt[:, 44:46], in1=rt[:, 40:42], op=ALU.add), reads=[rt], writes=[rt])
            S.op("dve", lambda e: e.tensor_tensor(out=rt[:, 44:46], in0=rt[:, 44:46], in1=rt[:, 30:32], op=ALU.mult), reads=[rt], writes=[rt])
            S.op("dve", lambda e: e.scalar_tensor_tensor(out=rt[:, 44:46], in0=rt[:, 36:38], scalar=DRV[:, 48:49], in1=rt[:, 44:46], op0=ALU.mult, op1=ALU.add),
                 reads=[rt, DRV], writes=[rt])
            S.op("dve", lambda e: e.scalar_tensor_tensor(out=rt[:, 46:48], in0=rt[:, 28:30], scalar=float(CAP), in1=rt[:, 26:28], op0=ALU.mult, op1=ALU.add),
                 reads=[rt], writes=[rt])
            S.op("dve", lambda e: e.tensor_tensor(out=rt[:, 46:48], in0=rt[:, 46:48], in1=rt[:, 30:32], op=ALU.mult), reads=[rt], writes=[rt])
            S.op("dve", lambda e: e.scalar_tensor_tensor(out=rt[:, 46:48], in0=rt[:, 36:38], scalar=float(NSLOT), in1=rt[:, 46:48], op0=ALU.mult, op1=ALU.add),
                 reads=[rt], writes=[rt])
            S.op("dve", lambda e, tile_i=tile_i: e.tensor_copy(out=ROWS[:, tile_i, :], in_=rt[:, 46:48]), reads=[rt], writes=[ROWS])
            yield
            S.op("dve", lambda e, tok0=tok0: e.tensor_scalar(out=rt[:, 48:49], in0=P("pidx"), scalar1=float(tok0), scalar2=None, op0=ALU.add),
                 reads=[PRM], writes=[rt])
            for k in range(2):
                en = ent[(tile_i * 2 + k) % 4]
                si = sidx[(tile_i * 2 + k) % 4]
                S.op("dve", lambda e, en=en: e.tensor_copy(out=en[:, 0:1], in_=rt[:, 48:49]), reads=[rt], writes=[en])
                S.op("dve", lambda e, en=en, k=k: e.tensor_copy(out=en[:, 1:2].bitcast(F32), in_=rt[:, 24 + k:25 + k]), reads=[rt], writes=[en])
                S.op("dve", lambda e, si=si, k=k: e.tensor_copy(out=si[:], in_=rt[:, 44 + k:45 + k]), reads=[rt], writes=[si])
                S.op("pool", lambda e, en=en, si=si: e.indirect_dma_start(out=tab.ap[:, :], out_offset=bass.IndirectOffsetOnAxis(ap=si[:, :], axis=0),
                                                                          in_=en[:, :], in_offset=None),
                     reads=[en, si, tab], dma=True)

        if st + 2 < NST:
            load_x(st + 2)
        yield

    load_x(0)
    if NST > 1:
        load_x(1)
    for _ in gen_M(0):
        pass
    for st in range(NST):
        gens = [gen_X(st)]
        if st + 1 < NST:
            gens.append(gen_M(st + 1))
        while gens:
            for g_ in list(gens):
                try:
                    for _ in range(BURST):
                        next(g_)
                except StopIteration:
                    gens.remove(g_)
    S.flush()
    S.pop()
    if stop in ("A", "A0"):
        S.pop()
        return nc, S

    S.push()
    TAB = S.sb([128, NB, 2], I32, "TAB")
    S.dma("sp", TAB, TAB[:].rearrange("p a b -> p (a b)"), tab, tab.ap[0:128 * NB, :].rearrange("(p a) b -> p (a b)", p=128))
    Wg = [S.sb([128, 8, DE], BF16, f"wg{i}") for i in range(2)]
    Wu = [S.sb([128, 8, DE], BF16, f"wu{i}") for i in range(2)]
    Wd = [S.sb([128, 4, D], BF16, f"wd{i}") for i in range(2)]
    xb = [S.sb([128, D], BF16, f"xb{i}") for i in range(2 * CB)]
    xbT2 = [S.sb([128, 8, CAP], BF16, f"xbT{i}") for i in range(2)]
    hT = S.sb([128, 4, CAP], BF16, "hT")
    th = [S.sb([128, 512], F32, f"th{i}") for i in range(2)]
    t2 = [S.sb([128, 512], F32, f"t2{i}") for i in range(2)]
    yo = [S.sb([128, D], F32, f"yo{i}") for i in range(2)]
    PT2 = [S.ps([128, 1024], BF16, f"pt2{i}") for i in range(2)]
    PG = [S.ps([128, 512], F32, f"pg{i}") for i in range(2)]
    PU = [S.ps([128, 512], F32, f"pu{i}") for i in range(2)]
    PD = [S.ps([128, 512], F32, f"pd{i}") for i in range(2)]

    def load_w(e):
        i = e % 2
        S.dma("pool", Wg[i], Wg[i][:], None, w_g_d[e].rearrange("(c p) n -> p c n", p=128))
        S.dma("pool", Wu[i], Wu[i][:], None, w_u_d[e].rearrange("(c p) n -> p c n", p=128))
        S.dma("pool", Wd[i], Wd[i][:], None, w_d_d[e].rearrange("(c p) n -> p c n", p=128))

    halves = []
    o = 0
    while o < CAP:
        n = min(512, CAP - o)
        halves.append((o, n))
        o += n
    def gather_x(ex):
        for b in range(CB):
            blk = ex * CB + b
            xg = xb[blk % (2 * CB)]
            S.op("pool", lambda e, xg=xg, blk=blk: e.indirect_dma_start(out=xg[:, :], out_offset=None, in_=n3buf.ap[:, :],
                                                                        in_offset=bass.IndirectOffsetOnAxis(ap=TAB[:, blk, 0:1], axis=0)),
                 reads=[TAB, n3buf], writes=[xg], dma=True)

    gi_c = [0]

    def transpose_block(ex, b):
        blk = ex * CB + b
        xg = xb[blk % (2 * CB)]
        gi = gi_c[0]
        gi_c[0] += 1
        pt = PT2[gi % 2]
        dst = xbT2[ex % 2]
        for k in range(8):
            S.op("pe", lambda e, k=k: e.transpose(out=pt[:, k * 128:(k + 1) * 128], in_=xg[:, k * 128:(k + 1) * 128], identity=identb[:]),
                 reads=[xg, identb], writes=[pt])
        if gi % 2 == 0:
            S.op("act", lambda e: e.copy(out=dst[:, :, b * 128:(b + 1) * 128], in_=pt[:].rearrange("p (k t) -> p k t", k=8)),
                 reads=[pt], writes=[dst])
        else:
            S.op("dve", lambda e: e.tensor_copy(out=dst[:, :, b * 128:(b + 1) * 128], in_=pt[:].rearrange("p (k t) -> p k t", k=8)),
                 reads=[pt], writes=[dst])

    gather_x(0)
    load_w(0)
    for b in range(CB):
        transpose_block(0, b)
    for ex in range(NEXP):
        if ex + 1 < NEXP:
            gather_x(ex + 1)
            load_w(ex + 1)
        wi = ex % 2
        xbT = xbT2[ex % 2]
        nxt = list(range(CB)) if ex + 1 < NEXP else []
        hi = 0
        for f in range(4):
            for (o0, n) in halves:
                pg = PG[hi % 2]
                pu = PU[hi % 2]
                tt = th[hi % 2]
                t22 = t2[hi % 2]
                hi += 1
                for k in range(8):
                    S.op("pe", lambda e, k=k, f=f, o0=o0, n=n, pg=pg: e.matmul(pg[:, 0:n], lhsT=Wg[wi][:, k, f * 128:(f + 1) * 128], rhs=xbT[:, k, o0:o0 + n],
                                                                               start=(k == 0), stop=(k == 7)), reads=[Wg[wi], xbT], writes=[pg])
                for k in range(8):
                    S.op("pe", lambda e, k=k, f=f, o0=o0, n=n, pu=pu: e.matmul(pu[:, 0:n], lhsT=Wu[wi][:, k, f * 128:(f + 1) * 128], rhs=xbT[:, k, o0:o0 + n],
                                                                               start=(k == 0), stop=(k == 7)), reads=[Wu[wi], xbT], writes=[pu])
                S.op("act", lambda e, n=n, pg=pg, tt=tt: e.activation(out=tt[:, 0:n], in_=pg[:, 0:n], func=AF.Tanh, scale=0.5), reads=[pg], writes=[tt])
                S.op("dve", lambda e, n=n, pg=pg, tt=tt, t22=t22: e.scalar_tensor_tensor(out=t22[:, 0:n], in0=tt[:, 0:n], scalar=1.0, in1=pg[:, 0:n],
                                                                                         op0=ALU.add, op1=ALU.mult), reads=[tt, pg], writes=[t22])
                S.op("dve", lambda e, n=n, pu=pu, t22=t22, f=f, o0=o0: e.scalar_tensor_tensor(out=hT[:, f, o0:o0 + n], in0=t22[:, 0:n], scalar=0.5, in1=pu[:, 0:n],
                                                                                              op0=ALU.mult, op1=ALU.mult), reads=[t22, pu], writes=[hT])
                if nxt:
                    transpose_block(ex + 1, nxt.pop(0))
        while nxt:
            transpose_block(ex + 1, nxt.pop(0))
        for b in range(CB):
            blk = ex * CB + b
            y = yo[blk % 2]
            for hf in range(2):
                for f in range(4):
                    S.op("pe", lambda e, f=f, hf=hf, b=b: e.matmul(PD[hf][:, :], lhsT=hT[:, f, b * 128:(b + 1) * 128],
                                                                   rhs=Wd[wi][:, f, hf * 512:(hf + 1) * 512], start=(f == 0), stop=(f == 3)),
                         reads=[hT, Wd[wi]], writes=[PD[hf]])
                if hf == 0:
                    S.op("act", lambda e, y=y, blk=blk, hf=hf: e.activation(out=y[:, hf * 512:(hf + 1) * 512], in_=PD[hf][:, :], func=AF.Copy,
                                                                            scale=TAB[:, blk, 1:2].bitcast(F32)),
                         reads=[PD[hf], TAB], writes=[y])
                else:
                    S.op("dve", lambda e, y=y, blk=blk, hf=hf: e.tensor_scalar(out=y[:, hf * 512:(hf + 1) * 512], in0=PD[hf][:, :],
                                                                               scalar1=TAB[:, blk, 1:2].bitcast(F32), scalar2=None, op0=ALU.mult),
                         reads=[PD[hf], TAB], writes=[y])
            S.dma("sp", None, ybuf.ap[blk * 128:(blk + 1) * 128, :], y, y[:], extra_reads=[ybuf])
    S.flush()
    S.pop()
    if stop == "B":
        S.pop()
        return nc, S

    S.push()
    NBUF = 4
    hb = [S.sb([128, D], F32, f"hb{i}") for i in range(NBUF)]
    y0 = [S.sb([128, D], F32, f"y0{i}") for i in range(NBUF)]
    y1 = [S.sb([128, D], F32, f"y1{i}") for i in range(NBUF)]
    ob = [S.sb([128, D], F32, f"ob{i}") for i in range(NBUF)]
    jk = S.sb([128, D], BF16, "jk")
    GF = S.sb([128, D], F32, "gF")
    S.dma("sp", GF, GF[:], None, gF_d[:, :])
    ssc = [S.sb([128, 1], F32, f"ssc{i}") for i in range(NBUF)]
    def c_load(t):
        i = t % NBUF
        S.dma("sp", hb[i], hb[i][:], h2buf, h2buf.ap[t * 128:(t + 1) * 128, :])
        for k, yy in enumerate((y0[i], y1[i])):
            S.op("pool", lambda e, yy=yy, t=t, k=k: e.indirect_dma_start(out=yy[:, :], out_offset=None, in_=ybuf.ap[:, :],
                                                                         in_offset=bass.IndirectOffsetOnAxis(ap=ROWS[:, t, k:k + 1], axis=0)),
                 reads=[ROWS, ybuf], writes=[yy], dma=True)

    for t in range(min(NBUF - 1, NT)):
        c_load(t)
    for t in range(NT):
        i = t % NBUF
        if t + NBUF - 1 < NT:
            c_load(t + NBUF - 1)
        S.op("dve", lambda e, i=i: e.tensor_tensor(out=hb[i][:], in0=hb[i][:], in1=y0[i][:], op=ALU.add), reads=[hb[i], y0[i]], writes=[hb[i]])
        S.op("dve", lambda e, i=i: e.tensor_tensor(out=hb[i][:], in0=hb[i][:], in1=y1[i][:], op=ALU.add), reads=[hb[i], y1[i]], writes=[hb[i]])
        S.op("act", lambda e, i=i: e.activation(out=jk[:], in_=hb[i][:], func=AF.Square, accum_out=ssc[i][:, 0:1]), reads=[hb[i]], writes=[jk, ssc[i]])
        rstd_from_ss(ssc[i], ssc[i], D)
        S.op("dve", lambda e, i=i: e.scalar_tensor_tensor(out=ob[i][:], in0=hb[i][:], scalar=ssc[i][:, 0:1], in1=GF[:], op0=ALU.mult, op1=ALU.mult),
             reads=[hb[i], ssc[i], GF], writes=[ob[i]])
        S.dma("sp", None, out_d[t * 128:(t + 1) * 128, :], ob[i], ob[i][:])
    S.flush()
    S.pop()
    S.pop()
    return nc, S


_CACHE = {}


def make_in_maps(inputs, SEQ, CB):
    inp = {k: np.asarray(v) for k, v in inputs.items()}
    offs, prm = pack_params(inp, CB * 128)
    l = 0
    w_r = np.ascontiguousarray(np.concatenate([inp["w_router_group"][l], inp["w_router_expert"][l]], axis=1), dtype=np.float32)
    shared = {
        "prm": prm,
        "w_in": np.ascontiguousarray(inp["w_in"][l], dtype=np.float32),
        "w_out": np.ascontiguousarray(inp["w_out"][l], dtype=np.float32),
        "w_q": np.ascontiguousarray(inp["w_q"][l], dtype=np.float32),
        "w_kv": np.ascontiguousarray(inp["w_kv"][l], dtype=np.float32),
        "w_o": np.ascontiguousarray(inp["w_o"][l], dtype=np.float32),
        "w_r": w_r,
        "w_gate": np.ascontiguousarray(inp["w_gate"][l], dtype=np.float32),
        "w_up": np.ascontiguousarray(inp["w_up"][l], dtype=np.float32),
        "w_down": np.ascontiguousarray(inp["w_down"][l], dtype=np.float32),
        "g3b": np.ascontiguousarray(_bc(inp["norm_moe"][l])),
        "gFb": np.ascontiguousarray(_bc(inp["norm_final"])),
    }
    B = inp["x"].shape[0]
    maps = []
    for b in range(B):
        m = dict(shared)
        m["x"] = np.ascontiguousarray(inp["x"][b], dtype=np.float32)
        m["mem"] = np.ascontiguousarray(inp["mem"][b], dtype=np.float32)
        maps.append(m)
    return offs, prm.shape[1], maps


def kernel(**inputs):
    SEQ = int(np.asarray(inputs["x"]).shape[1])
    TS = 256
    CB = max(1, (SEQ * 2 // NEXP) * 3 // 2 // 128)
    offs, nprm, maps = make_in_maps(inputs, SEQ, CB)
    key = (SEQ, TS, CB)
    if key not in _CACHE:
        _CACHE[key] = build(SEQ, TS, CB, offs, nprm)[0]
    nc = _CACHE[key]
    res = run_bass_kernel_spmd(nc, maps, core_ids=list(range(len(maps))))
    out = np.stack([np.asarray(r["out"], dtype=np.float32) for r in res.results], axis=0)
    return out
```
